# Optimizing a Trainium2 kernel written in Bass

```python
import jax
import jax.numpy as jnp
from jax import lax
import numpy as np

D_MODEL = 1024
BATCH = 8
SEQ = 2048
DEPTH = 2

N_A_LAYERS = DEPTH // 2
N_B_LAYERS = DEPTH - N_A_LAYERS

A_HEAD_DIM = 128
A_HEADS = D_MODEL // A_HEAD_DIM
A_WIDTH = A_HEADS * A_HEAD_DIM
A_CONV = 4
A_CHUNK = 64
A_PROJ = 4 * A_WIDTH + 2 * A_HEADS

B_HEAD_DIM = 64
B_Q_HEADS = D_MODEL // B_HEAD_DIM
B_KV_HEADS = max(1, B_Q_HEADS // 8)
B_GROUP = B_Q_HEADS // B_KV_HEADS
WINDOW = 128
ROPE_DIM = B_HEAD_DIM // 4
ROPE_THETA = 500000.0

N_EXPERTS = 32
TOP_K = 4
D_FF = D_MODEL
SWIGLU_LIMIT = 7.0
SWIGLU_ALPHA = 1.702
MOE_BLOCK = 128

EPS = 1e-6
F32 = jnp.float32

kernel_name = "yoco_gdn_swa_sink_moe_adaln"


def rms_norm(x, gain):
    xf = x.astype(F32)
    y = xf * lax.rsqrt(jnp.mean(xf * xf, axis=-1, keepdims=True) + EPS)
    return (y * gain.astype(F32)).astype(x.dtype)


def ada_norm(x, gain, shift, scale):
    return rms_norm(x, gain) * (1.0 + scale[:, None, :]) + shift[:, None, :]


def l2_norm(x):
    xf = x.astype(F32)
    return (xf * lax.rsqrt(jnp.sum(xf * xf, axis=-1, keepdims=True) + EPS)).astype(x.dtype)


def causal_depthwise_conv(x, w):
    k_len = w.shape[0]
    s_len = x.shape[1]
    xp = jnp.pad(x, ((0, 0), (k_len - 1, 0), (0, 0)))
    return sum(xp[:, i:i + s_len] * w[i] for i in range(k_len))


def partial_rope(x, positions):
    inv_freq = ROPE_THETA ** (-jnp.arange(0, ROPE_DIM, 2, dtype=F32) / ROPE_DIM)
    ang = positions.astype(F32)[..., None] * inv_freq
    cos = jnp.cos(ang)[:, :, None, :]
    sin = jnp.sin(ang)[:, :, None, :]
    x1, x2 = jnp.split(x[..., :ROPE_DIM].astype(F32), 2, axis=-1)
    rot = jnp.concatenate([x1 * cos - x2 * sin, x2 * cos + x1 * sin], axis=-1)
    return jnp.concatenate([rot.astype(x.dtype), x[..., ROPE_DIM:]], axis=-1)


def chunk_gated_delta_rule(q, k, v, g, beta):
    b_sz, s_len, n_h, d_k = q.shape
    d_v = v.shape[-1]
    n_c = s_len // A_CHUNK

    def chunks(t):
        t = t.astype(F32).reshape(b_sz, n_c, A_CHUNK, n_h, *t.shape[3:])
        return jnp.moveaxis(t, (1, 3), (0, 2))

    q, k, v, g, beta = map(chunks, (q, k, v, g, beta))
    gc = jnp.cumsum(g, axis=-1)
    idx = jnp.arange(A_CHUNK)
    incl = idx[:, None] >= idx[None, :]
    strict = idx[:, None] > idx[None, :]
    decay = jnp.exp(jnp.where(incl, gc[..., :, None] - gc[..., None, :], -jnp.inf))
    kb = k * beta[..., None]
    a_mat = jnp.where(strict, jnp.einsum('nbhid,nbhjd->nbhij', kb, k) * decay, 0.0)
    rhs = jnp.concatenate([v * beta[..., None], kb * jnp.exp(gc)[..., None]], axis=-1)
    sol = lax.linalg.triangular_solve(a_mat, rhs, left_side=True, lower=True, unit_diagonal=True)
    u, w = sol[..., :d_v], sol[..., d_v:]
    qk = jnp.einsum('nbhid,nbhjd->nbhij', q, k) * decay
    q_dec = q * jnp.exp(gc)[..., None]
    k_tail = k * jnp.exp(gc[..., -1:] - gc)[..., None]
    g_tot = jnp.exp(gc[..., -1])

    def step(state, xs):
        q_c, kt_c, u_c, w_c, qk_c, gt_c = xs
        v_new = u_c - jnp.einsum('bhcd,bhde->bhce', w_c, state)
        o_c = jnp.einsum('bhcd,bhde->bhce', q_c, state) + jnp.einsum('bhij,bhje->bhie', qk_c, v_new)
        state = state * gt_c[..., None, None] + jnp.einsum('bhcd,bhce->bhde', kt_c, v_new)
        return state, o_c

    s0 = jnp.zeros((b_sz, n_h, d_k, d_v), F32)
    _, o = lax.scan(step, s0, (q_dec, k_tail, u, w, qk, g_tot))
    return jnp.moveaxis(o, (0, 2), (1, 3)).reshape(b_sz, s_len, n_h, d_v)


def gated_deltanet(h, w_in, conv_w, a_log, dt_bias, out_gain, w_out):
    b_sz, s_len, _ = h.shape
    proj = h @ w_in
    qkv, z, a, b = jnp.split(proj, [3 * A_WIDTH, 4 * A_WIDTH, 4 * A_WIDTH + A_HEADS], axis=-1)
    qkv = jax.nn.silu(causal_depthwise_conv(qkv, conv_w))
    q, k, v = [t.reshape(b_sz, s_len, A_HEADS, A_HEAD_DIM) for t in jnp.split(qkv, 3, axis=-1)]
    q = l2_norm(q) * (A_HEAD_DIM ** -0.5)
    k = l2_norm(k)
    beta = jax.nn.sigmoid(b.astype(F32))
    g = -jnp.exp(a_log.astype(F32)) * jax.nn.softplus(a.astype(F32) + dt_bias.astype(F32))
    o = chunk_gated_delta_rule(q, k, v, g, beta).astype(h.dtype)
    o = rms_norm(o, out_gain) * jax.nn.silu(z.reshape(b_sz, s_len, A_HEADS, A_HEAD_DIM))
    return o.reshape(b_sz, s_len, A_WIDTH) @ w_out


def shared_kv(x, gain, shift, scale, w_kv, k_gain, positions):
    b_sz, s_len, _ = x.shape
    h = ada_norm(x, gain, shift, scale)
    k, v = jnp.split(h @ w_kv, 2, axis=-1)
    k = k.reshape(b_sz, s_len, B_KV_HEADS, B_HEAD_DIM)
    v = v.reshape(b_sz, s_len, B_KV_HEADS, B_HEAD_DIM)
    k = partial_rope(rms_norm(k, k_gain), positions)
    return k, v


def swa_sink_attention(h, k, v, w_q, q_gain, sinks, w_out, positions):
    b_sz, s_len, _ = h.shape
    n_b = s_len // WINDOW
    q = (h @ w_q).reshape(b_sz, s_len, B_Q_HEADS, B_HEAD_DIM)
    q = partial_rope(rms_norm(q, q_gain), positions)
    qb = q.reshape(b_sz, n_b, WINDOW, B_KV_HEADS, B_GROUP, B_HEAD_DIM)
    kb = k.reshape(b_sz, n_b, WINDOW, B_KV_HEADS, B_HEAD_DIM)
    vb = v.reshape(b_sz, n_b, WINDOW, B_KV_HEADS, B_HEAD_DIM)

    def with_prev(t):
        prev = jnp.concatenate([jnp.zeros_like(t[:, :1]), t[:, :-1]], axis=1)
        return jnp.concatenate([prev, t], axis=2)

    k_band, v_band = with_prev(kb), with_prev(vb)
    s = jnp.einsum('bnqhgd,bnkhd->bnhgqk', qb, k_band, preferred_element_type=F32) * (B_HEAD_DIM ** -0.5)
    qi = jnp.arange(WINDOW)[:, None] + WINDOW
    kj = jnp.arange(2 * WINDOW)[None, :]
    band = (kj <= qi) & (qi - kj < WINDOW)
    has_prev = (jnp.arange(n_b) > 0)[:, None, None] | (kj >= WINDOW)[None]
    mask = band[None] & has_prev
    s = jnp.where(mask[None, :, None, None], s, -jnp.inf)
    sink = sinks.astype(F32).reshape(B_KV_HEADS, B_GROUP)[None, None, :, :, None, None]
    m = jnp.maximum(jnp.max(s, axis=-1, keepdims=True), sink)
    p = jnp.exp(s - m)
    p = p / (jnp.sum(p, axis=-1, keepdims=True) + jnp.exp(sink - m))
    o = jnp.einsum('bnhgqk,bnkhd->bnqhgd', p.astype(v.dtype), v_band)
    return o.reshape(b_sz, s_len, B_Q_HEADS * B_HEAD_DIM) @ w_out


def moe_ffn(h, router_w, router_b, up_w, up_b, down_w, down_b):
    b_sz, s_len, d = h.shape
    n_tok = b_sz * s_len
    xt = h.reshape(n_tok, d)
    logits = jnp.dot(xt, router_w, preferred_element_type=F32) + router_b.astype(F32)
    top_logit, top_idx = lax.top_k(logits, TOP_K)
    gates = jax.nn.softmax(top_logit, axis=-1)
    n_as = n_tok * TOP_K
    flat_e = top_idx.reshape(-1).astype(jnp.int32)
    order = jnp.argsort(flat_e)
    sorted_e = flat_e[order]
    counts = jnp.bincount(flat_e, length=N_EXPERTS).astype(jnp.int32)
    starts = jnp.cumsum(counts) - counts
    padded = (counts + MOE_BLOCK - 1) // MOE_BLOCK * MOE_BLOCK
    p_ends = jnp.cumsum(padded)
    p_starts = p_ends - padded
    rank = jnp.arange(n_as, dtype=jnp.int32) - starts[sorted_e]
    dest_sorted = p_starts[sorted_e] + rank
    n_rows = n_as + N_EXPERTS * MOE_BLOCK
    n_blk = n_rows // MOE_BLOCK
    row_token = jnp.full((n_rows,), n_tok, jnp.int32).at[dest_sorted].set((order // TOP_K).astype(jnp.int32))
    x_pad = jnp.concatenate([xt, jnp.zeros((1, d), xt.dtype)], axis=0)
    xb = x_pad[row_token].reshape(n_blk, MOE_BLOCK, d)
    blk_start = jnp.arange(n_blk, dtype=jnp.int32) * MOE_BLOCK
    blk_expert = jnp.clip(jnp.searchsorted(p_ends, blk_start, side='right'), 0, N_EXPERTS - 1)

    def expert_block(args):
        xblk, e = args
        gu = xblk @ up_w[e] + up_b[e]
        gate, lin = jnp.split(gu, 2, axis=-1)
        gate = jnp.minimum(gate, SWIGLU_LIMIT)
        lin = jnp.clip(lin, -SWIGLU_LIMIT, SWIGLU_LIMIT)
        act = gate * jax.nn.sigmoid(SWIGLU_ALPHA * gate) * (lin + 1.0)
        return act @ down_w[e] + down_b[e]

    y_rows = lax.map(expert_block, (xb, blk_expert)).reshape(n_rows, d)
    dest = jnp.zeros((n_as,), jnp.int32).at[order].set(dest_sorted)
    y = jnp.einsum('tkd,tk->td', y_rows[dest].reshape(n_tok, TOP_K, d), gates.astype(h.dtype))
    return y.reshape(b_sz, s_len, d)


def _normal(k, shape, scale):
    return jax.random.normal(k, shape, F32) * scale


def setup_inputs(seed: int = 0) -> dict:
    key = jax.random.key(seed)
    ks = jax.random.split(key, 32)
    d = D_MODEL
    x = _normal(ks[0], (BATCH, SEQ, d), 1.0)
    c = _normal(ks[1], (BATCH, d), 1.0)
    offset = jax.random.randint(ks[2], (BATCH, 1), 0, 4096, dtype=jnp.int32)
    positions = offset + jnp.arange(SEQ, dtype=jnp.int32)[None, :]

    ada_w = _normal(ks[3], (DEPTH, d, 6 * d), 0.5 * d ** -0.5)
    ada_b = _normal(ks[4], (DEPTH, 6 * d), 0.02)
    norm_gain = 1.0 + _normal(ks[5], (DEPTH, 2, d), 0.02)

    a_w_in = _normal(ks[6], (N_A_LAYERS, d, A_PROJ), d ** -0.5)
    a_conv = _normal(ks[7], (N_A_LAYERS, A_CONV, 3 * A_WIDTH), A_CONV ** -0.5)
    a_log = jnp.log(jax.random.uniform(ks[8], (N_A_LAYERS, A_HEADS), F32, 1.0, 16.0))
    dt = jnp.exp(jax.random.uniform(ks[9], (N_A_LAYERS, A_HEADS), F32, np.log(1e-3), np.log(1e-1)))
    a_dt_bias = dt + jnp.log(-jnp.expm1(-dt))
    a_out_gain = 1.0 + _normal(ks[10], (N_A_LAYERS, A_HEAD_DIM), 0.02)
    a_w_out = _normal(ks[11], (N_A_LAYERS, A_WIDTH, d), A_WIDTH ** -0.5)

    kv_ada_w = _normal(ks[12], (d, 2 * d), 0.5 * d ** -0.5)
    kv_ada_b = _normal(ks[13], (2 * d,), 0.02)
    kv_norm_gain = 1.0 + _normal(ks[14], (d,), 0.02)
    kv_w = _normal(ks[15], (d, 2 * B_KV_HEADS * B_HEAD_DIM), d ** -0.5)
    k_norm_gain = 1.0 + _normal(ks[16], (B_HEAD_DIM,), 0.02)

    b_w_q = _normal(ks[17], (N_B_LAYERS, d, B_Q_HEADS * B_HEAD_DIM), d ** -0.5)
    q_norm_gain = 1.0 + _normal(ks[18], (N_B_LAYERS, B_HEAD_DIM), 0.02)
    b_sinks = _normal(ks[19], (N_B_LAYERS, B_Q_HEADS), 0.5)
    b_w_out = _normal(ks[20], (N_B_LAYERS, B_Q_HEADS * B_HEAD_DIM, d), (B_Q_HEADS * B_HEAD_DIM) ** -0.5)

    router_w = _normal(ks[21], (DEPTH, d, N_EXPERTS), d ** -0.5)
    router_b = _normal(ks[22], (DEPTH, N_EXPERTS), 0.01)
    up_w = _normal(ks[23], (DEPTH, N_EXPERTS, d, 2 * D_FF), d ** -0.5)
    up_b = _normal(ks[24], (DEPTH, N_EXPERTS, 2 * D_FF), 0.01)
    down_w = _normal(ks[25], (DEPTH, N_EXPERTS, D_FF, d), D_FF ** -0.5)
    down_b = _normal(ks[26], (DEPTH, N_EXPERTS, d), 0.01)
    return {
        "x": x, "c": c, "positions": positions,
        "ada_w": ada_w, "ada_b": ada_b, "norm_gain": norm_gain,
        "a_w_in": a_w_in, "a_conv": a_conv, "a_log": a_log, "a_dt_bias": a_dt_bias,
        "a_out_gain": a_out_gain, "a_w_out": a_w_out,
        "kv_ada_w": kv_ada_w, "kv_ada_b": kv_ada_b, "kv_norm_gain": kv_norm_gain,
        "kv_w": kv_w, "k_norm_gain": k_norm_gain,
        "b_w_q": b_w_q, "q_norm_gain": q_norm_gain, "b_sinks": b_sinks, "b_w_out": b_w_out,
        "router_w": router_w, "router_b": router_b, "up_w": up_w, "up_b": up_b,
        "down_w": down_w, "down_b": down_b,
    }


def reference(x, c, positions, ada_w, ada_b, norm_gain, a_w_in, a_conv, a_log, a_dt_bias,
              a_out_gain, a_w_out, kv_ada_w, kv_ada_b, kv_norm_gain, kv_w, k_norm_gain,
              b_w_q, q_norm_gain, b_sinks, b_w_out, router_w, router_b, up_w, up_b,
              down_w, down_b):
    c_act = jax.nn.silu(c)
    k_sh, v_sh = None, None
    for layer in range(DEPTH):
        if layer == N_A_LAYERS:
            kv_shift, kv_scale = jnp.split(c_act @ kv_ada_w + kv_ada_b, 2, axis=-1)
            k_sh, v_sh = shared_kv(x, kv_norm_gain, kv_shift, kv_scale, kv_w, k_norm_gain, positions)
        mod = c_act @ ada_w[layer] + ada_b[layer]
        sh1, sc1, g1, sh2, sc2, g2 = jnp.split(mod, 6, axis=-1)
        h = ada_norm(x, norm_gain[layer, 0], sh1, sc1)
        if layer < N_A_LAYERS:
            y = gated_deltanet(h, a_w_in[layer], a_conv[layer], a_log[layer], a_dt_bias[layer],
                               a_out_gain[layer], a_w_out[layer])
        else:
            j = layer - N_A_LAYERS
            y = swa_sink_attention(h, k_sh, v_sh, b_w_q[j], q_norm_gain[j], b_sinks[j],
                                   b_w_out[j], positions)
        x = x + g1[:, None, :] * y
        h = ada_norm(x, norm_gain[layer, 1], sh2, sc2)
        y = moe_ffn(h, router_w[layer], router_b[layer], up_w[layer], up_b[layer],
                    down_w[layer], down_b[layer])
        x = x + g2[:, None, :] * y
    return x
```

```python
import numpy as np
from contextlib import ExitStack
import concourse.bass as bass
import concourse.mybir as mybir
from concourse.bass_utils import run_bass_kernel_spmd

F32 = mybir.dt.float32
BF16 = mybir.dt.bfloat16
I32 = mybir.dt.int32
U32 = mybir.dt.uint32
AF = mybir.ActivationFunctionType
ALU = mybir.AluOpType
ENGINES = ("pe", "act", "dve", "pool", "sp")
NEG = -60000.0


class V:
    __slots__ = ("t", "ap")

    def __init__(self, t, ap):
        self.t, self.ap = t, ap

    def __getitem__(self, k):
        return V(self.t, self.ap[k])

    def bitcast(self, dt):
        return V(self.t, self.ap.bitcast(dt))

    def rearrange(self, s, **kw):
        return V(self.t, self.ap.rearrange(s, **kw))

    def bcast(self, shape):
        return V(self.t, self.ap.to_broadcast(list(shape)))


class T:
    _n = 0

    def __init__(self, ap, name):
        self.ap0 = ap
        self.name = name
        self.id = T._n
        T._n += 1
        self.last_writer = None
        self.readers = []
        self.psum = False

    def __getitem__(self, k):
        return V(self, self.ap0[k])

    @property
    def v(self):
        return V(self, self.ap0)


class Op:
    __slots__ = ("eng", "fn", "reads", "writes", "is_dma", "key", "deps", "signal",
                 "tok_sem", "tok_val", "idx", "sid")


class Prog:
    def __init__(self, nc):
        self.nc = nc
        self.ops = []
        self.stack = ExitStack()
        self.pending = {}
        self.last_eng_op = {}
        self.last_key_op = {}
        self.arena = None
        self.arena_off = 0
        self.arena_words = 0
        self.scope_id = 0

    def make_arena(self, words):
        self.arena = self.stack.enter_context(self.nc.sbuf_tensor("arena", [128, words], F32))
        self.arena_words = words

    def scope(self):
        deps = set(self.last_eng_op.values()) | set(self.last_key_op.values())
        self.pending = {e: set(deps) for e in ENGINES}
        self.arena_off = 0
        self.scope_id += 1

    def sc(self, name, shape, dtype=F32):
        n = 1
        for d in shape[1:]:
            n *= d
        words = (n + 1) // 2 if dtype == BF16 else n
        words = (words + 7) // 8 * 8
        assert self.arena_off + words <= self.arena_words, ("arena overflow", name, self.arena_off + words)
        ap = self.arena[0:shape[0], self.arena_off:self.arena_off + words]
        self.arena_off += words
        if dtype != F32:
            ap = ap.bitcast(dtype)
        ap = ap[:, 0:n]
        if len(shape) == 3:
            ap = ap.rearrange("p (a b) -> p a b", a=shape[1])
        return T(ap, name)

    def sb(self, name, shape, dtype=F32):
        t = self.stack.enter_context(self.nc.sbuf_tensor(name, list(shape), dtype))
        return T(t[tuple(slice(None) for _ in shape)], name)

    def ps(self, name, shape, dtype=F32):
        t = self.stack.enter_context(self.nc.psum_tensor(name, list(shape), dtype))
        tt = T(t[tuple(slice(None) for _ in shape)], name)
        tt.psum = True
        return tt

    def dram(self, name, shape, dtype=F32, kind="Internal"):
        t = self.nc.dram_tensor(name, list(shape), dtype, kind=kind)
        return T(t.ap(), name)

    def _add(self, eng, fn, reads, writes, is_dma=False, key=None):
        o = Op()
        o.eng, o.fn, o.is_dma, o.key = eng, fn, is_dma, key
        o.reads = list({t.id: t for t in reads}.values())
        o.writes = list({t.id: t for t in list(writes) + [t for t in reads if t.psum]}.values())
        o.deps, o.signal = set(), False
        i = len(self.ops)
        o.idx = i
        o.sid = self.scope_id
        for t in o.reads:
            if t.last_writer is not None:
                o.deps.add(t.last_writer)
        for t in o.writes:
            if t.last_writer is not None:
                o.deps.add(t.last_writer)
            o.deps.update(t.readers)
        for t in o.reads:
            t.readers.append(i)
        for t in o.writes:
            t.last_writer = i
            t.readers = []
        if self.pending.get(eng):
            o.deps |= self.pending.pop(eng)
        if is_dma:
            self.last_key_op[key.id] = i
        else:
            self.last_eng_op[eng] = i
        o.deps.discard(i)
        self.ops.append(o)
        return o

    def I(self, eng, fn, ins, outs):
        return self._add(eng, fn, [v.t for v in ins], [v.t for v in outs])

    def dma(self, eng, out, in_, key=None, **kw):
        k = key if key is not None else (out.t if out.t.name[0] != "@" else in_.t)
        return self._add(eng, lambda e: e.dma_start(out=out.ap, in_=in_.ap, **kw), [in_.t], [out.t],
                         is_dma=True, key=k)

    def mm(self, out, lhsT, rhs, start=True, stop=True, extra_reads=()):
        return self.I("pe", lambda e: e.matmul(out.ap, lhsT=lhsT.ap, rhs=rhs.ap, start=start, stop=stop),
                      [lhsT, rhs] + ([] if start else [out]) + list(extra_reads), [out])

    def tr(self, out, in_, ident):
        return self.I("pe", lambda e: e.transpose(out=out.ap, in_=in_.ap, identity=ident.ap), [in_, ident], [out])

    def act(self, out, in_, func, bias=None, scale=None, accum=None, eng="act"):
        ins = [in_] + ([bias] if isinstance(bias, V) else []) + ([scale] if isinstance(scale, V) else [])
        outs = [out] + ([accum] if accum is not None else [])

        def fn(e):
            kw = {}
            if bias is not None:
                kw["bias"] = bias.ap if isinstance(bias, V) else bias
            if scale is not None:
                kw["scale"] = scale.ap if isinstance(scale, V) else scale
            if accum is not None:
                kw["accum_out"] = accum.ap
            return e.activation(out=out.ap, in_=in_.ap, func=func, **kw)
        return self.I(eng, fn, ins, outs)

    def ts(self, out, in0, s1, op0, s2=None, op1=None, eng="dve"):
        ins = [in0] + [s for s in (s1, s2) if isinstance(s, V)]

        def fn(e):
            a1 = s1.ap if isinstance(s1, V) else s1
            a2 = s2.ap if isinstance(s2, V) else s2
            if op1 is None:
                return e.tensor_scalar(out=out.ap, in0=in0.ap, scalar1=a1, scalar2=None, op0=op0)
            return e.tensor_scalar(out=out.ap, in0=in0.ap, scalar1=a1, scalar2=a2, op0=op0, op1=op1)
        return self.I(eng, fn, ins, [out])

    def tt(self, out, in0, in1, op, eng="dve"):
        return self.I(eng, lambda e: e.tensor_tensor(out=out.ap, in0=in0.ap, in1=in1.ap, op=op), [in0, in1], [out])

    def stt(self, out, in0, scalar, in1, op0, op1):
        ins = [in0, in1] + ([scalar] if isinstance(scalar, V) else [])
        return self.I("dve", lambda e: e.scalar_tensor_tensor(
            out=out.ap, in0=in0.ap, scalar=(scalar.ap if isinstance(scalar, V) else scalar),
            in1=in1.ap, op0=op0, op1=op1), ins, [out])

    def copy(self, out, in_, eng="dve"):
        if eng == "act":
            return self.act(out, in_, AF.Copy)
        return self.I(eng, lambda e: e.tensor_copy(out=out.ap, in_=in_.ap), [in_], [out])

    def memset(self, out, val, eng="pool"):
        return self.I(eng, lambda e: e.memset(out.ap, val), [], [out])

    def recip(self, out, in_):
        return self.I("dve", lambda e: e.reciprocal(out=out.ap, in_=in_.ap), [in_], [out])

    def affsel(self, out, in_, pattern, cmp, fill, base, cm):
        return self.I("pool", lambda e: e.affine_select(out=out.ap, in_=in_.ap, pattern=pattern, compare_op=cmp,
                                                        fill=fill, base=base, channel_multiplier=cm), [in_], [out])

    def emit(self):
        nc, ops = self.nc, self.ops
        for o in ops:
            keep = set()
            for d in o.deps:
                p = ops[d]
                if not p.is_dma and not o.is_dma and p.eng == o.eng:
                    if o.eng == "pe":
                        continue
                keep.add(d)
            o.deps = keep
            for d in keep:
                ops[d].signal = True
        eng_cnt = {e: 0 for e in ENGINES}
        key_cnt, sems = {}, {}
        slot_of, nslot = {}, {}
        for o in ops:
            if o.is_dma:
                sw = o.eng == "pool"
                sk = (o.sid, o.key.id, sw)
                if sk not in slot_of:
                    n = nslot.get((o.sid, sw), 0)
                    slot_of[sk] = (sw, n)
                    nslot[(o.sid, sw)] = n + 1
                k = slot_of[sk]
                key_cnt[k] = key_cnt.get(k, 0) + 16
                o.tok_sem, o.tok_val = ("k", k), key_cnt[k]
            elif o.signal:
                eng_cnt[o.eng] += 1
                o.tok_sem, o.tok_val = ("e", o.eng), eng_cnt[o.eng]
        for e in ENGINES:
            sems[("e", e)] = self.stack.enter_context(nc.semaphore("sem_" + e))
        for k in key_cnt:
            sems[("k", k)] = self.stack.enter_context(nc.semaphore("semk_%d_%d" % (int(k[0]), k[1])))
        self.n_sems = len(sems)
        final = [(("k", k), v) for k, v in key_cnt.items()] + [(("e", e), v) for e, v in eng_cnt.items() if v > 0]
        with nc.Block() as block:
            def mk(en):
                def body(eng):
                    waited = {}
                    for o in ops:
                        if o.eng != en:
                            continue
                        need = {}
                        for d in o.deps:
                            p = ops[d]
                            if p.tok_val > need.get(p.tok_sem, 0):
                                need[p.tok_sem] = p.tok_val
                        for s, v in need.items():
                            if waited.get(s, 0) >= v:
                                continue
                            eng.wait_ge(sems[s], v)
                            waited[s] = v
                        ins = o.fn(eng)
                        if o.is_dma:
                            ins.then_inc(sems[o.tok_sem], 16)
                        elif o.signal:
                            ins.then_inc(sems[o.tok_sem], 1)
                    if en == "sp":
                        for s, v in final:
                            if waited.get(s, 0) < v:
                                eng.wait_ge(sems[s], v)
                return body
            block.tensor(mk("pe"))
            block.scalar(mk("act"))
            block.vector(mk("dve"))
            block.gpsimd(mk("pool"))
            block.sync(mk("sp"))
        self.stack.close()


D = 1024
KC = 8
EPS = 1e-6
ARENA_WORDS = 19840


class _Cut(Exception):
    pass


def build(S=2048, NE=32, stop_after=None, cut=None):
    NT = S // 128
    TQ = min(S, 512)
    NQ = S // TQ
    CPQ = TQ // 128
    nc = bass.Bass("TRN2", target_bir_lowering=False)
    P = Prog(nc)

    def din(name, shape, dtype=F32):
        t = P.dram(name, shape, dtype, kind="ExternalInput")
        t.name = "@" + name
        return t

    x_in = din("x", [S, D]); cT_in = din("cT", [128, KC]); pos_in = din("pos", [128, NT], I32)
    ada_w = din("ada_w", [2, D, 6 * D]); ada_b = din("ada_b", [2, 6 * D]); norm_gain = din("norm_gain", [4, D])
    a_w_in = din("a_w_in", [D, 4112]); a_convT = din("a_convT", [3072, 4]); a_log = din("a_log", [8])
    a_dt_bias = din("a_dt_bias", [8]); a_out_gain = din("a_out_gain", [128]); a_w_out = din("a_w_out", [D, D])
    kv_ada_w = din("kv_ada_w", [D, 2 * D]); kv_ada_b = din("kv_ada_b", [1, 2 * D]); kv_norm_gain = din("kv_norm_gain", [1, D])
    kv_w = din("kv_w", [D, 256]); k_norm_gain = din("k_norm_gain", [64]); b_w_q = din("b_w_q", [D, D])
    q_norm_gain = din("q_norm_gain", [64]); b_sinks = din("b_sinks", [16]); b_w_out = din("b_w_out", [D, D])
    router_w = din("router_w", [2, D, NE]); router_b = din("router_b", [2, NE])
    up_w = din("up_w", [2, NE, D, 2 * D]); up_bT = din("up_bT", [2, NE, 128, 16])
    down_w = din("down_w", [2, NE, D, D]); down_b = din("down_b", [2, NE, D])
    y_out = P.dram("y", [S, D], F32, kind="ExternalOutput"); y_out.name = "@y"

    def ck(n):
        if cut is not None and cut == n:
            raise _Cut()

    def wview(v):
        v = v.v if isinstance(v, T) else v
        return v.rearrange("(kc p) f -> p kc f", p=128)

    def pbc(t, idx=None):
        ap = t.ap0 if idx is None else t.ap0[idx]
        return V(t, ap.partition_broadcast(128))

    xs = [P.sb("x%d" % i, [128, D]) for i in range(NT)]
    hT = P.sb("hT", [128, KC, S], BF16)
    ident = P.sb("ident", [128, 128]); ones = P.sb("ones", [128, 128])
    Uincl = P.sb("Uincl", [128, 128])
    MA = P.sb("MA", [128, 128]); zeros = P.sb("zeros", [128, 128])
    m01 = P.sb("m01", [128, 256], BF16)
    modt = [P.sb("mod%d" % j, [128, D]) for j in range(3)]
    crep = P.sb("crep", [128, KC, 128], BF16)
    cact = P.sb("cact", [128, KC])
    rowb = P.sb("rowb", [1, 512]); rowg = P.sb("rowg", [1, 512])
    NPC = 4
    piece = []
    pc_i = [0]

    def alloc_pieces():
        piece[:] = [P.sc("piece%d" % i, [128, KC, 512], BF16) for i in range(NPC)]
        pc_i[0] = 0
    hrow = P.sb("hrow", [128, D]); htmp = P.sb("htmp", [128, D]); ssq = P.sb("ssq", [128, 4])
    gates = [P.sb("gates%d" % i, [128, NE]) for i in range(NT)]
    gks = [P.sb("gks%d" % i, [128, 4]) for i in range(NT)]
    idxs = [P.sb("idxs%d" % i, [128, 4], I32) for i in range(NT)]
    zeros_row = P.sb("zeros_row", [1, 512])
    xg = P.dram("xg", [NE * (S // 2) + 1, D], BF16); xg.name = "@xg"
    yg = P.dram("yg", [NE * (S // 2) + 1, D], F32); yg.name = "@yg"
    P.make_arena(ARENA_WORDS)

    def next_piece():
        p = piece[pc_i[0] % NPC]
        pc_i[0] += 1
        return p
    pb = [P.ps("pb%d" % i, [128, 512]) for i in range(8)]
    pb_i = [0]

    def next_pb():
        p = pb[pb_i[0] % 8]
        pb_i[0] += 1
        return p
    ev_i = [0]

    def evac_eng():
        ev_i[0] += 1
        return "act" if ev_i[0] % 2 else "dve"

    P.memset(ones.v, 1.0); P.memset(zeros.v, 0.0); P.memset(zeros_row.v, 0.0)
    P.affsel(ident.v, ones.v, [[1, 128]], ALU.is_equal, 0.0, 0, -1)
    P.affsel(Uincl.v, ones.v, [[1, 128]], ALU.is_ge, 0.0, 0, -1)
    P.affsel(MA.v, zeros.v, [[-1, 128]], ALU.is_gt, NEG, 0, 1)
    P.affsel(hrow[:, 0:128], ones.v, [[-1, 128]], ALU.is_gt, 0.0, 0, 1)
    P.copy(m01[:, 0:128], hrow[:, 0:128], eng="pool")
    P.copy(m01[:, 128:256], Uincl.v, eng="pool")

    for i in range(NT):
        P.dma("sp", xs[i].v, x_in[i * 128:(i + 1) * 128, :])
    P.dma("sp", cact.v, cT_in.v)
    P.act(cact.v, cact.v, AF.Silu)
    for kc in range(KC):
        P.ts(crep[:, kc, :], ones.v, cact[:, kc:kc + 1], ALU.mult)

    def compute_mod(wdram, brow, col0, outs):
        for n, o in enumerate(outs):
            c0 = col0 + n * 512
            pc = next_piece()
            P.dma("pool", pc.v, wview(wdram)[:, :, c0:c0 + 512])
            P.dma("sp", rowb.v, brow[0:1, c0:c0 + 512])
            ps = next_pb()
            P.mm(ps.v, ones[0:1, :], rowb.v, True, False)
            for kc in range(KC):
                P.mm(ps.v, crep[:, kc, :], pc[:, kc, :], False, kc == KC - 1)
            P.copy(o, ps.v, eng=evac_eng())

    def gain_fold(dst, grow):
        for hh in range(2):
            P.dma("sp", rowg.v, grow[0:1, hh * 512:(hh + 1) * 512])
            ps = next_pb()
            P.mm(ps.v, ones[0:1, :], rowg.v, True, True)
            P.stt(dst[:, hh * 512:(hh + 1) * 512], dst[:, hh * 512:(hh + 1) * 512], 1.0, ps.v, ALU.add, ALU.mult)

    def layer_mod(l, half):
        P.scope()
        alloc_pieces()
        outs = []
        for j in range(3):
            outs += [modt[j][:, 0:512], modt[j][:, 512:1024]]
        compute_mod(ada_w[l], ada_b[l:l + 1, :], half * 3 * D, outs)
        gain_fold(modt[1], norm_gain[2 * l + half:2 * l + half + 1, :])

    def rms_rstd(dst, src_sum, n):
        P.ts(dst, src_sum, 1.0 / n, ALU.mult, EPS, ALU.add)
        P.act(dst, dst, AF.Sqrt)
        P.recip(dst, dst)

    def norm_mod_tile(i, A, B, out_f32):
        P.act(htmp.v, xs[i].v, AF.Square, accum=ssq[:, 0:1])
        rms_rstd(ssq[:, 1:2], ssq[:, 0:1], D)
        P.stt(htmp.v, xs[i].v, ssq[:, 1:2], A.v, ALU.mult, ALU.mult)
        P.tt(out_f32, htmp.v, B.v, ALU.add, eng="pool")

    def transpose8(src_f32, dsts):
        for half in range(2):
            ps = next_pb()
            for q in range(4):
                kc = half * 4 + q
                P.tr(ps[:, q * 128:(q + 1) * 128], src_f32[:, kc * 128:(kc + 1) * 128], ident.v)
            for dv, eng in dsts:
                P.copy(dv[:, half * 4:half * 4 + 4, :], ps.v.rearrange("p (q t) -> p q t", q=4), eng=eng)

    def norm_all(A, B):
        for i in range(NT):
            norm_mod_tile(i, A, B, hrow.v)
            transpose8(hrow.v, [(hT[:, :, i * 128:(i + 1) * 128], evac_eng())])

    bc_cache = {}

    def bc_reg(e, val):
        if val not in bc_cache:
            bc_cache[val] = e.to_reg(val)
        return bc_cache[val]

    def moe(l):
        B, A, G = modt
        CAP = S // 2
        NB = CAP // 128
        NH = max(CAP // 512, 1)
        HW_ = min(CAP, 512)
        TRASH = NE * CAP
        P.scope()
        h2T_f = P.sc("h2Tf", [128, KC, 128]); rw_sb = P.sc("rw_sb", [128, KC, NE]); rbrep = P.sc("rbrep", [128, NE])
        lg = P.sc("lg", [128, NE]); lgr = P.sc("lgr", [128, NE]); top8 = P.sc("top8", [128, 8]); msk = P.sc("msk", [128, NE])
        sm = P.sc("sm", [128, 4]); idx8 = P.sc("idx8", [128, 8], U32); ef = P.sc("ef", [128, 4])
        db_sb = P.sc("db_sb", [NE, D]); gT = P.sc("gT", [NE, 128]); gpad = P.sc("gpad", [128, 128])
        runsum = P.sc("runsum", [128, NE]); pos = P.sc("pos", [128, NE]); ov = P.sc("ov", [128, NE]); t1 = P.sc("t1", [128, NE])
        iota_e = P.sc("iota_e", [128, NE]); base_e = P.sc("base_e", [128, NE]); iota_i = P.sc("iota_i", [128, NE], I32)
        oh = P.sc("oh", [128, NE]); ohj = P.sc("ohj", [128, NE]); destf = P.sc("destf", [128, 4])
        Ustr = P.sc("Ustr", [128, 128])
        h2b = [P.sc("h2b%d" % j, [128, D], BF16) for j in range(2)]
        zt = P.sc("zt", [128, 4096], BF16)
        P.memset(gpad.v, 0.0)
        P.memset(runsum.v, 0.0)
        P.tt(Ustr.v, Uincl.v, ident.v, ALU.subtract, eng="pool")
        for e_ in range(NE):
            P.memset(base_e[:, e_:e_ + 1], float(e_ * CAP), eng="pool")
        if l == 0:
            P.memset(zt.v, 0.0, eng="pool")
            rows_per = 128 * 4
            for r0 in range(0, NE * CAP, rows_per):
                P.dma("sp", xg[r0:r0 + rows_per, :].rearrange("(p a) d -> p (a d)", p=128), zt.v, key=zt)
            P.dma("sp", xg[TRASH:TRASH + 1, :], zt[0:1, 0:D], key=zt)
            P.copy(lg[0:1, 0:NE], zt[0:1, 0:NE])
            for q in range(2):
                P.dma("sp", yg[TRASH:TRASH + 1, q * 512:(q + 1) * 512], zeros_row.v, key=zeros_row)
        P.dma("sp", rw_sb.v, wview(router_w[l]))
        P.dma("sp", rbrep.v, pbc(router_b, l))
        P.dma("sp", db_sb.v, down_b[l])
        P.tt(db_sb.v, db_sb.v, G[0:NE, :], ALU.mult, eng="pool")
        for i in range(NT):
            norm_mod_tile(i, A, B, hrow.v)
            hb = h2b[i % 2]
            P.copy(hb.v, hrow.v, eng="pool")
            transpose8(hrow.v, [(h2T_f.v, evac_eng())])
            ps = next_pb()
            for kc in range(KC):
                P.mm(ps[:, 0:NE], h2T_f[:, kc, :], rw_sb[:, kc, :], kc == 0, kc == KC - 1)
            P.tt(lgr.v, ps[:, 0:NE], rbrep.v, ALU.add)
            P.I("dve", lambda e: e.max(out=top8.v.ap, in_=lgr.v.ap), [lgr.v], [top8.v])
            P.ts(msk.v, lgr.v, top8[:, 3:4], ALU.is_ge)
            P.ts(sm[:, 0:1], top8[:, 0:1], -1.0, ALU.mult)
            P.act(lg.v, lgr.v, AF.Exp, bias=sm[:, 0:1], scale=1.0)
            P.tt(lg.v, lg.v, msk.v, ALU.mult)
            P.I("dve", lambda e: e.reduce_sum(out=sm[:, 1:2].ap, in_=lg.v.ap, axis=mybir.AxisListType.X),
                [lg.v], [sm[:, 1:2]])
            P.recip(sm[:, 2:3], sm[:, 1:2])
            P.ts(gpad[:, 0:NE], lg.v, sm[:, 2:3], ALU.mult)
            P.copy(gates[i].v, gpad[:, 0:NE], eng="pool")
            P.act(gks[i].v, top8[:, 0:4], AF.Exp, bias=sm[:, 0:1], scale=1.0)
            P.ts(gks[i].v, gks[i].v, sm[:, 2:3], ALU.mult)
            ps = next_pb()
            P.tr(ps[:, 0:128], gpad.v, ident.v)
            P.copy(gT.v, ps[0:NE, 0:128], eng="act")
            for hh in range(2):
                ps = next_pb()
                P.mm(ps.v, gT.v, db_sb[:, hh * 512:(hh + 1) * 512], True, True)
                P.tt(xs[i][:, hh * 512:(hh + 1) * 512], xs[i][:, hh * 512:(hh + 1) * 512], ps.v, ALU.add)
            ps = next_pb()
            P.mm(ps[:, 0:NE], Ustr.v, msk.v, True, False)
            P.mm(ps[:, 0:NE], ones.v, runsum.v, False, True)
            P.copy(pos.v, ps[:, 0:NE], eng="act")
            P.tt(runsum.v, runsum.v, msk.v, ALU.add, eng="pool")
            P.ts(ov.v, pos.v, float(CAP), ALU.is_ge)
            P.tt(pos.v, pos.v, base_e.v, ALU.add)
            P.tt(t1.v, pos.v, ov.v, ALU.mult)
            P.tt(pos.v, pos.v, t1.v, ALU.subtract)
            P.stt(pos.v, ov.v, float(TRASH), pos.v, ALU.mult, ALU.add)
            for k in range(4):
                P.ts(oh.v, lgr.v, top8[:, k:k + 1], ALU.is_equal)
                P.tt(ohj.v, oh.v, pos.v, ALU.mult)
                P.I("dve", lambda e, k=k: e.reduce_sum(out=destf[:, k:k + 1].ap, in_=ohj.v.ap, axis=mybir.AxisListType.X),
                    [ohj.v], [destf.v])
            P.copy(idxs[i].v, destf.v)
            for k in range(4):
                P._add("pool", (lambda e, k=k, hb=hb, ix=idxs[i]: e.indirect_dma_start(
                    out=xg.ap0[:, :], out_offset=bass.IndirectOffsetOnAxis(ap=ix[:, k:k + 1].ap, axis=0),
                    in_=hb.v.ap, in_offset=None, bounds_check=bc_reg(e, TRASH), oob_is_err=False)),
                    [hb, idxs[i]], [xg], is_dma=True, key=hb)
        P.scope()
        alloc_pieces()
        xT = T(hT.ap0[:, :, 0:CAP], "xT"); actT = T(hT.ap0[:, :, CAP:2 * CAP], "actT")
        xin = [P.sc("xin%d" % j, [128, D], BF16) for j in range(NB)]

        def load_x(e_):
            for b_ in range(NB):
                P.dma("sp", xin[b_].v, xg[e_ * CAP + b_ * 128: e_ * CAP + (b_ + 1) * 128, :])
        ub = P.sc("ub", [128, 16]); ub1 = P.sc("ub1", [128, 8])
        gact = [P.sc("gact%d" % j, [128, HW_]) for j in range(2)]
        sgm = [P.sc("sgm%d" % j, [128, HW_], BF16) for j in range(2)]
        gsb = [P.sc("gsb%d" % j, [128, HW_], BF16) for j in range(2)]
        lin1 = [P.sc("lin1%d" % j, [128, HW_]) for j in range(2)]
        ybuf = [P.sc("ybuf%d" % j, [128, D]) for j in range(2)]
        identb = P.sc("identb", [128, 128], BF16)
        P.copy(identb.v, ident.v, eng="pool")
        cnt = 0
        load_x(0)
        for e in range(NE):
            P.dma("sp", ub.v, up_bT[l, e])
            P.ts(ub1.v, ub[:, 8:16], 1.0, ALU.add)
            for b in range(NB):
                xi = xin[b]
                ps = next_pb()
                psb = ps.v.bitcast(BF16)
                for kc in range(KC):
                    P.tr(psb[:, kc * 128:(kc + 1) * 128], xi[:, kc * 128:(kc + 1) * 128], identb.v)
                P.copy(xT[:, :, b * 128:(b + 1) * 128], psb.rearrange("p (k t) -> p k t", k=KC), eng=evac_eng())
            if e + 1 < NE:
                load_x(e + 1)
            pds = []
            for quad in range(2):
                pg, pl = next_piece(), next_piece()
                P.dma("pool", pg.v, wview(up_w[l, e])[:, :, quad * 512:(quad + 1) * 512])
                P.dma("pool", pl.v, wview(up_w[l, e])[:, :, 1024 + quad * 512:1024 + (quad + 1) * 512])
                for mm_ in range(4):
                    m = quad * 4 + mm_
                    for hf in range(NH):
                        ssl = slice(hf * HW_, (hf + 1) * HW_)
                        bb = cnt % 2
                        cnt += 1
                        psg, psl = next_pb(), next_pb()
                        for kc in range(KC):
                            P.mm(psg[:, 0:HW_], pg[:, kc, mm_ * 128:(mm_ + 1) * 128], xT[:, kc, ssl], kc == 0, kc == KC - 1)
                        for kc in range(KC):
                            P.mm(psl[:, 0:HW_], pl[:, kc, mm_ * 128:(mm_ + 1) * 128], xT[:, kc, ssl], kc == 0, kc == KC - 1)
                        P.ts(gact[bb].v, psg[:, 0:HW_], ub[:, m:m + 1], ALU.add, 7.0, ALU.min)
                        P.act(sgm[bb].v, gact[bb].v, AF.Sigmoid, scale=1.702)
                        P.tt(gsb[bb].v, gact[bb].v, sgm[bb].v, ALU.mult)
                        P.ts(lin1[bb].v, psl[:, 0:HW_], ub1[:, m:m + 1], ALU.add, 8.0, ALU.min)
                        P.stt(actT[:, m, ssl], lin1[bb].v, -6.0, gsb[bb].v, ALU.max, ALU.mult)
            for quad in range(2):
                pd = next_piece()
                dsrc = down_w[l, e][quad * 512:(quad + 1) * 512, :].rearrange("(c p) f -> p c f", p=128)
                pdv = pd.v.rearrange("p a b -> p (a b)").rearrange("p (c f) -> p c f", c=4)
                P.dma("pool", pdv, dsrc)
                for c4 in range(4):
                    for hh in range(2):
                        P.tt(pdv[:, c4, hh * 512:(hh + 1) * 512], pdv[:, c4, hh * 512:(hh + 1) * 512],
                             G[:, hh * 512:(hh + 1) * 512], ALU.mult)
                pds.append(pdv)
            for b in range(NB):
                yb = ybuf[b % 2]
                for hh in range(2):
                    ps = next_pb()
                    for m in range(8):
                        P.mm(ps.v, actT[:, m, b * 128:(b + 1) * 128], pds[m // 4][:, m % 4, hh * 512:(hh + 1) * 512],
                             m == 0, m == 7)
                    P.copy(yb[:, hh * 512:(hh + 1) * 512], ps.v, eng=evac_eng())
                P.dma("sp", yg[e * CAP + b * 128: e * CAP + (b + 1) * 128, :], yb.v, key=yb)
        P.scope()
        yks = [[P.sc("yk%d_%d" % (r, j), [128, D]) for j in range(4)] for r in range(3)]
        for i in range(NT):
            yk = yks[i % 3]
            for k in range(4):
                P._add("pool", (lambda e, k=k, ix=idxs[i], yk=yk: e.indirect_dma_start(
                    out=yk[k].v.ap, out_offset=None, in_=yg.ap0[:, :],
                    in_offset=bass.IndirectOffsetOnAxis(ap=ix[:, k:k + 1].ap, axis=0), bounds_check=bc_reg(e, TRASH), oob_is_err=False)),
                    [yg, idxs[i]], [yk[k]], is_dma=True, key=yk[k])
            for k in range(4):
                P.stt(xs[i].v, yk[k].v, gks[i][:, k:k + 1], xs[i].v, ALU.mult, ALU.add)

    H = 8
    NCH = NT
    LNSC = float(np.log(128.0 ** -0.5))

    def gdn():
        B, A, G = modt
        norm_all(A, B)
        ck(1)
        P.scope()
        sq = lambda n: P.sc(n, [128, 128])
        ab_sb = P.sc("ab_sb", [128, KC, 16], BF16)
        sc_a = P.sc("sc_a", [128, NCH, 8]); sc_beta = P.sc("sc_beta", [128, NCH, 8]); sc_nbeta = P.sc("sc_nbeta", [128, NCH, 8])
        sc_g = P.sc("sc_g", [128, NCH, 8]); sc_gc = P.sc("sc_gc", [128, NCH, 8]); sc_egc = P.sc("sc_egc", [128, NCH, 8])
        sc_etail = P.sc("sc_etail", [128, NCH, 8]); sc_gtot = P.sc("sc_gtot", [128, NCH, 8]); sc_ng = P.sc("sc_ng", [128, NCH, 8])
        dtb = P.sc("dtb", [128, 8]); alog = P.sc("alog", [128, 8]); gainrep = sq("gainrep")
        TQG = 128; NQG = S // TQG; CPQG = TQG // 128

        def mkbufs(j):
            n = lambda s_: "%s_%d" % (s_, j)
            sqj = lambda s_: P.sc(n(s_), [128, 128])
            return ([P.sc(n("wqh%d" % q), [128, KC, 128], BF16) for q in range(4)], P.sc(n("wout_h"), [128, D], BF16),
                    P.sc(n("convw"), [128, 3, 4]), [P.sc(n("pre%d" % q), [128, TQG + 3]) for q in range(3)],
                    P.sc(n("cvt"), [128, TQG]), [P.sc(n("qkv%d" % q), [128, TQG]) for q in range(3)],
                    sqj("sqb"), P.sc(n("rn"), [128, 8]), P.sc(n("fac"), [128, 8]),
                    sqj("gU"), sqj("gUq"), sqj("gUk"), sqj("Elow"), sqj("Eincl"), sqj("egq"),
                    sqj("Nm0"), sqj("NmT0"), sqj("PTm0"),
                    sqj("QKm"), sqj("QKmT"), sqj("qdT"), sqj("vb"), sqj("kbg"), sqj("ktail"),
                    sqj("Sst"), sqj("o_sb"), sqj("z_sb"), sqj("og"),
                    P.sc(n("ogT"), [128, 128], BF16))
        KH = 3
        bufs = [mkbufs(j) for j in range(KH)]
        P.dma("pool", ab_sb.v, wview(a_w_in)[:, :, 4096:4112])
        P.dma("sp", dtb.v, pbc(a_dt_bias)); P.dma("sp", alog.v, pbc(a_log)); P.dma("sp", gainrep.v, pbc(a_out_gain))
        P.act(alog.v, alog.v, AF.Exp)
        ck(2)
        for c in range(NCH):
            ps = next_pb()
            for kc in range(KC):
                P.mm(ps[:, 0:16], hT[:, kc, c * 128:(c + 1) * 128], ab_sb[:, kc, :], kc == 0, kc == KC - 1)
            P.tt(sc_a[:, c, :], ps[:, 0:8], dtb.v, ALU.add)
            P.act(sc_beta[:, c, :], ps[:, 8:16], AF.Sigmoid)
        allc = lambda t: t.v.rearrange("p c h -> p (c h)")
        P.act(allc(sc_a), allc(sc_a), AF.Exp)
        P.act(allc(sc_a), allc(sc_a), AF.Ln, bias=1.0, scale=1.0)
        for c in range(NCH):
            P.stt(sc_g[:, c, :], sc_a[:, c, :], -1.0, alog.v, ALU.mult, ALU.mult)
        P.ts(allc(sc_ng), allc(sc_g), -1.0, ALU.mult)
        P.ts(allc(sc_nbeta), allc(sc_beta), -1.0, ALU.mult)
        for c in range(NCH):
            ps = next_pb()
            P.mm(ps[:, 0:8], Uincl.v, sc_g[:, c, :], True, True)
            P.mm(ps[:, 8:16], ones.v, sc_g[:, c, :], True, True)
            P.copy(sc_gc[:, c, :], ps[:, 0:8], eng="dve")
            P.act(sc_egc[:, c, :], ps[:, 0:8], AF.Exp)
            P.act(sc_gtot[:, c, :], ps[:, 8:16], AF.Exp)
            P.tt(sc_etail[:, c, :], ps[:, 8:16], sc_gc[:, c, :], ALU.subtract)
        P.act(allc(sc_etail), allc(sc_etail), AF.Exp)
        ck(3)
        def head_gen(h, b):
            (wq_h, wout_h, convw, pre, cvt, qkv, sqb, rn, fac, gU, gUq, gUk, Elow, Eincl, egq, Nm0, NmT0, PTm0,
             QKm, QKmT, qdT, vb, kbg, ktail, Sst, o_sb, z_sb, og, ogT) = b
            Nm = [Nm0, gU]; NmT = [NmT0, gUq]; PTm = [PTm0, gUk]
            u_sb, wT, vnew = Elow, Eincl, egq
            wout_f = wout_h
            for j in range(4):
                P.dma("pool", wq_h[j].v, wview(a_w_in)[:, :, j * 1024 + h * 128: j * 1024 + (h + 1) * 128])
            P.dma("sp", convw.v, V(a_convT, a_convT.ap0.rearrange("(j h p) k -> h p j k", j=3, p=128)[h]))
            P.dma("pool", wout_h.v, a_w_out[h * 128:(h + 1) * 128, :])
            P.tt(wout_f.v, wout_h.v, G.v, ALU.mult, eng="pool")
            P.memset(Sst.v, 0.0, eng="pool")
            ck(4)
            for tq in range(NQG):
                for j in range(3):
                    if tq == 0:
                        P.memset(pre[j][:, 0:3], 0.0, eng="pool")
                    else:
                        P.copy(pre[j][:, 0:3], pre[j][:, TQG:TQG + 3], eng="pool")
                    ps = next_pb()
                    for kc in range(KC):
                        P.mm(ps[:, 0:TQG], wq_h[j][:, kc, :], hT[:, kc, tq * TQG:(tq + 1) * TQG], kc == 0, kc == KC - 1)
                    P.copy(pre[j][:, 3:3 + TQG], ps[:, 0:TQG], eng="act")
                    P.ts(cvt.v, pre[j][:, 0:TQG], convw[:, j, 0:1], ALU.mult)
                    for k in range(1, 4):
                        P.stt(cvt.v, pre[j][:, k:k + TQG], convw[:, j, k:k + 1], cvt.v, ALU.mult, ALU.add)
                    P.act(qkv[j].v, cvt.v, AF.Silu)
                    yield
                ck(5)
                for cc in range(CPQG):
                    c = tq * CPQG + cc
                    lsl = slice(cc * 128, (cc + 1) * 128)
                    csl = slice(c * 128, (c + 1) * 128)
                    qc, kcn, vc = qkv[0][:, lsl], qkv[1][:, lsl], qkv[2][:, lsl]
                    bcol = lambda t: t[:, c, h:h + 1]
                    ps = next_pb()
                    P.act(sqb.v, qc, AF.Square)
                    P.mm(ps[:, 0:1], sqb.v, ones[:, 0:1], True, True)
                    P.act(sqb.v, kcn, AF.Square)
                    P.mm(ps[:, 1:2], sqb.v, ones[:, 0:1], True, True)
                    P.ts(rn[:, 0:2], ps[:, 0:2], EPS, ALU.add)
                    yield
                    P.act(rn[:, 4:6], rn[:, 0:2], AF.Ln)
                    P.ts(rn[:, 4:5], rn[:, 4:5], -0.5, ALU.mult, LNSC, ALU.add)
                    P.ts(rn[:, 5:6], rn[:, 5:6], -0.5, ALU.mult)
                    P.act(rn[:, 2:4], rn[:, 4:6], AF.Exp)
                    ck(6)
                    P.tt(fac[:, 0:1], bcol(sc_nbeta), rn[:, 3:4], ALU.mult, eng="pool")
                    P.tt(fac[:, 1:2], bcol(sc_beta), bcol(sc_egc), ALU.mult, eng="pool")
                    P.tt(fac[:, 1:2], fac[:, 1:2], rn[:, 3:4], ALU.mult, eng="pool")
                    P.tt(fac[:, 2:3], bcol(sc_etail), rn[:, 3:4], ALU.mult, eng="pool")
                    yield
                    P.ts(gU.v, Uincl.v, bcol(sc_ng), ALU.mult, eng="pool")
                    P.stt(gUk.v, ident.v, rn[:, 5:6], gU.v, ALU.mult, ALU.add)
                    P.ts(gUq.v, Uincl.v, bcol(sc_g), ALU.mult, eng="pool")
                    P.stt(gUq.v, ident.v, rn[:, 4:5], gUq.v, ALU.mult, ALU.add)
                    yield
                    p1 = next_pb()
                    P.mm(p1[:, 0:128], ones.v, gUk.v, True, False)
                    P.mm(p1[:, 0:128], ident.v, MA.v, False, True)
                    P.mm(p1[:, 128:256], ones.v, gUq.v, True, True)
                    yield
                    P.act(Elow.v, p1[:, 0:128], AF.Exp, bias=bcol(sc_gc), scale=1.0)
                    P.act(egq.v, p1[:, 128:256], AF.Exp)
                    P.tt(qdT.v, qc, egq.v, ALU.mult, eng="pool")
                    P.stt(Eincl.v, ident.v, rn[:, 3:4], Elow.v, ALU.mult, ALU.add)
                    yield
                    ck(7)
                    p2 = next_pb()
                    P.mm(p2[:, 0:128], kcn, kcn, True, True)
                    P.mm(p2[:, 128:256], qc, kcn, True, True)
                    yield
                    P.stt(Nm[0].v, p2[:, 0:128], fac[:, 0:1], Elow.v, ALU.mult, ALU.mult)
                    P.stt(QKm.v, p2[:, 128:256], rn[:, 2:3], Eincl.v, ALU.mult, ALU.mult)
                    yield
                    ck(71)
                    p3 = next_pb()
                    P.tr(p3[:, 0:128], Nm[0].v, ident.v)
                    P.tr(p3[:, 128:256], QKm.v, ident.v)
                    P.tr(p3[:, 256:384], kcn, ident.v)
                    P.tr(p3[:, 384:512], vc, ident.v)
                    yield
                    ck(72)
                    P.copy(NmT[0].v, p3[:, 0:128], eng="act")
                    P.copy(QKmT.v, p3[:, 128:256], eng="act")
                    P.tt(PTm[0].v, p3[:, 0:128], ident.v, ALU.add)
                    ck(73)
                    P.act(kbg.v, p3[:, 256:384], AF.Copy, scale=fac[:, 1:2])
                    P.act(ktail.v, p3[:, 256:384], AF.Copy, scale=fac[:, 2:3])
                    P.ts(vb.v, p3[:, 384:512], bcol(sc_beta), ALU.mult)
                    yield
                    ck(8)
                    cur = 0
                    for lev in range(6):
                        nxt = 1 - cur
                        p4 = next_pb()
                        P.mm(p4[:, 0:128], NmT[cur].v, Nm[cur].v, True, True)
                        if lev < 5:
                            P.mm(p4[:, 128:256], Nm[cur].v, NmT[cur].v, True, True)
                        yield
                        P.copy(Nm[nxt].v, p4[:, 0:128], eng="act")
                        if lev < 5:
                            P.copy(NmT[nxt].v, p4[:, 128:256], eng="dve")
                        yield
                        p5 = next_pb()
                        P.mm(p5[:, 0:128], Nm[nxt].v, PTm[cur].v, True, True)
                        yield
                        P.tt(PTm[nxt].v, p5[:, 0:128], PTm[cur].v, ALU.add)
                        yield
                        cur = nxt
                    ck(9)
                    PT = PTm[cur]
                    p6 = next_pb()
                    P.mm(p6[:, 0:128], PT.v, vb.v, True, True)
                    P.mm(p6[:, 128:256], kbg.v, PT.v, True, True)
                    yield
                    P.copy(u_sb.v, p6[:, 0:128], eng="act")
                    P.copy(wT.v, p6[:, 128:256], eng="dve")
                    yield
                    p7 = next_pb()
                    P.mm(p7[:, 0:128], wT.v, Sst.v, True, True)
                    yield
                    P.tt(vnew.v, u_sb.v, p7[:, 0:128], ALU.subtract)
                    yield
                    P.mm(p7[:, 128:256], qdT.v, Sst.v, True, False)
                    P.mm(p7[:, 128:256], QKmT.v, vnew.v, False, True)
                    P.mm(p7[:, 256:384], ktail.v, vnew.v, True, True)
                    yield
                    P.copy(o_sb.v, p7[:, 128:256], eng="act")
                    P.stt(Sst.v, Sst.v, bcol(sc_gtot), p7[:, 256:384], ALU.mult, ALU.add)
                    ck(10)
                    p8 = next_pb()
                    for kc in range(KC):
                        P.mm(p8[:, 0:128], hT[:, kc, csl], wq_h[3][:, kc, :], kc == 0, kc == KC - 1)
                    P.act(z_sb.v, p8[:, 0:128], AF.Silu)
                    yield
                    P.act(sqb.v, o_sb.v, AF.Square, accum=rn[:, 6:7])
                    rms_rstd(rn[:, 7:8], rn[:, 6:7], 128)
                    P.stt(og.v, o_sb.v, rn[:, 7:8], gainrep.v, ALU.mult, ALU.mult)
                    P.tt(og.v, og.v, z_sb.v, ALU.mult, eng="pool")
                    yield
                    P.tr(p8[:, 128:256], og.v, ident.v)
                    P.copy(ogT.v, p8[:, 128:256], eng="act")
                    yield
                    for hh in range(2):
                        ps = next_pb()
                        P.mm(ps.v, ogT.v, wout_f[:, hh * 512:(hh + 1) * 512], True, True)
                        P.tt(xs[c][:, hh * 512:(hh + 1) * 512], xs[c][:, hh * 512:(hh + 1) * 512], ps.v, ALU.add)

        for h0 in range(0, H, KH):
            gens = [head_gen(h0 + j, bufs[j]) for j in range(KH) if h0 + j < H]
            while gens:
                for g_ in list(gens):
                    try:
                        next(g_)
                    except StopIteration:
                        gens.remove(g_)

    TWO_PI = float(2 * np.pi)
    PI = float(np.pi)

    def layer1_mixer():
        B, A, G = modt
        P.scope()
        alloc_pieces()
        kTd = P.sc("kTd", [128, 2, S], BF16)
        vext = [P.sc("vext%d" % i, [128, 2, 65], BF16) for i in range(NT)]
        kgain = P.sc("kgain", [128, 64]); qgain = P.sc("qgain", [128, 64]); sinkrep = P.sc("sinkrep", [128, 16])
        posf = P.sc("posf", [128, NT]); posi = P.sc("posi", [128, NT], I32)
        cosb = P.sc("cosb", [128, NT, 8]); sinb = P.sc("sinb", [128, NT, 8])
        ang = P.sc("ang", [128, NT, 8]); angk = P.sc("angk", [128, NT, 8]); angi = P.sc("angi", [128, NT, 8], I32)
        invf = P.sc("invf", [128, NT, 8])
        kvw = P.sc("kvw", [128, KC, 256], BF16)
        kdup = P.sc("kdup", [128, 2, 128])
        kvt = P.sc("kvt", [128, 256]); qt = P.sc("qt", [128, D]); qsq = P.sc("qsq", [128, D])
        kvmod = [qt, qsq]
        hs = P.sc("hs", [128, 16]); rot = P.sc("rot", [128, 16, 16]); rtmp = P.sc("rtmp", [128, 16, 8])
        qTn = P.sc("qTn", [128, KC, 128], BF16)
        pexp = [P.sc("pexp%d" % i, [128, 8, 256], BF16) for i in range(2)]
        o_t = htmp; den = P.sc("den", [128, 16]); oT = P.sc("oT", [128, KC, 128], BF16)

        def rope_tables():
            P.dma("sp", posi.v, pos_in.v)
            P.copy(posf.v, posi.v)
            for f in range(8):
                P.memset(invf[:, :, f:f + 1], float(500000.0 ** (-(2 * f) / 16.0)), eng="pool")
            for shift, dst in ((0.0, sinb), (PI / 2, cosb)):
                P.tt(ang.v, invf.v, posf.v.rearrange("p (t o) -> p t o", o=1).bcast([128, NT, 8]), ALU.mult)
                if shift:
                    P.ts(ang.v, ang.v, shift, ALU.add)
                P.ts(angk.v, ang.v, 1.0 / TWO_PI, ALU.mult)
                P.copy(angi.v, angk.v)
                P.copy(angk.v, angi.v)
                P.stt(ang.v, angk.v, -TWO_PI, ang.v, ALU.mult, ALU.add)
                P.ts(angk.v, ang.v, PI, ALU.is_gt)
                P.stt(ang.v, angk.v, -TWO_PI, ang.v, ALU.mult, ALU.add)
                P.ts(angk.v, ang.v, -PI, ALU.is_lt)
                P.stt(ang.v, angk.v, TWO_PI, ang.v, ALU.mult, ALU.add)
                P.ts(ang.v, ang.v, PI, ALU.min, -PI, ALU.max)
                P.act(dst.v, ang.v, AF.Sin)

        def rms_rope(src, nh, gain, i, dst):
            s3 = src.rearrange("p (h d) -> p h d", d=64)
            d3 = dst.rearrange("p (h d) -> p h d", d=64)
            q3 = qsq[:, 0:nh * 64].rearrange("p (h d) -> p h d", d=64)
            P.tt(qsq[:, 0:nh * 64], src, src, ALU.mult, eng="pool")
            P.I("dve", lambda e: e.reduce_sum(out=hs[:, 0:nh].ap, in_=q3.ap, axis=mybir.AxisListType.X), [q3], [hs.v])
            rms_rstd(hs[:, 0:nh], hs[:, 0:nh], 64)
            P.tt(d3, s3, hs[:, 0:nh].rearrange("p (h o) -> p h o", o=1).bcast([128, nh, 64]), ALU.mult)
            P.tt(d3, d3, gain.v.rearrange("p (o d) -> p o d", o=1).bcast([128, nh, 64]), ALU.mult)
            cb = cosb[:, i, :].rearrange("p (o f) -> p o f", o=1).bcast([128, nh, 8])
            sb_ = sinb[:, i, :].rearrange("p (o f) -> p o f", o=1).bcast([128, nh, 8])
            x1, x2 = d3[:, :, 0:8], d3[:, :, 8:16]
            r = rot[:, 0:nh, :]
            P.tt(r[:, :, 0:8], x1, cb, ALU.mult)
            P.tt(rtmp[:, 0:nh, :], x2, sb_, ALU.mult)
            P.tt(r[:, :, 0:8], r[:, :, 0:8], rtmp[:, 0:nh, :], ALU.subtract)
            P.tt(r[:, :, 8:16], x2, cb, ALU.mult)
            P.tt(rtmp[:, 0:nh, :], x1, sb_, ALU.mult)
            P.tt(r[:, :, 8:16], r[:, :, 8:16], rtmp[:, 0:nh, :], ALU.add)
            P.copy(d3[:, :, 0:16], r, eng="pool")

        compute_mod(kv_ada_w, kv_ada_b, 0, [kvmod[0][:, 0:512], kvmod[0][:, 512:1024], kvmod[1][:, 0:512], kvmod[1][:, 512:1024]])
        gain_fold(kvmod[1], kv_norm_gain)
        P.dma("pool", kvw.v, wview(kv_w))
        P.dma("sp", kgain.v, pbc(k_norm_gain)); P.dma("sp", qgain.v, pbc(q_norm_gain)); P.dma("sp", sinkrep.v, pbc(b_sinks))
        P.act(sinkrep.v, sinkrep.v, AF.Exp)
        rope_tables()
        norm_all(kvmod[1], kvmod[0])
        for i in range(NT):
            isl = slice(i * 128, (i + 1) * 128)
            ps = next_pb()
            for kc in range(KC):
                P.mm(ps[:, 0:256], hT[:, kc, isl], kvw[:, kc, :], kc == 0, kc == KC - 1)
            P.copy(kvt.v, ps[:, 0:256], eng="act")
            P.memset(vext[i].v, 1.0, eng="pool")
            P.copy(vext[i][:, :, 0:64], kvt[:, 128:256].rearrange("p (g d) -> p g d", g=2))
            rms_rope(kvt[:, 0:128], 2, kgain, i, qt[:, 0:128])
            k3 = qt[:, 0:128].rearrange("p (g d) -> p g d", g=2)
            P.copy(kdup[:, :, 0:64], k3, eng="pool")
            P.copy(kdup[:, :, 64:128], k3, eng="pool")
            ps = next_pb()
            for g in range(2):
                P.tr(ps[:, g * 128:(g + 1) * 128], kdup[:, g, :], ident.v)
            P.copy(kTd[:, :, isl], ps[:, 0:256].rearrange("p (g t) -> p g t", g=2), eng="act")
        norm_all(A, B)
        wq2 = [piece[0], piece[1]]; wo2 = [piece[2], piece[3]]
        for hh in range(2):
            P.dma("pool", wq2[hh].v, wview(b_w_q)[:, :, hh * 512:(hh + 1) * 512])
            P.dma("pool", wo2[hh].v, wview(b_w_out)[:, :, hh * 512:(hh + 1) * 512])
            for kc in range(KC):
                P.tt(wo2[hh][:, kc, :], wo2[hh][:, kc, :], G[:, hh * 512:(hh + 1) * 512], ALU.mult, eng="pool")
        m3 = m01.v.rearrange("p (o k) -> p o k", o=1).bcast([128, 8, 256])
        for n in range(NT):
            nsl = slice(n * 128, (n + 1) * 128)
            psl = slice((n - 1) * 128, n * 128) if n > 0 else nsl
            for hh in range(2):
                ps = next_pb()
                for kc in range(KC):
                    P.mm(ps.v, hT[:, kc, nsl], wq2[hh][:, kc, :], kc == 0, kc == KC - 1)
                P.copy(qt[:, hh * 512:(hh + 1) * 512], ps.v, eng="act")
            rms_rope(qt.v, 16, qgain, n, hrow.v)
            transpose8(hrow.v, [(qTn.v, evac_eng())])
            for g in range(2):
                pe_ = pexp[g]
                for quad in range(2):
                    pss = [next_pb(), next_pb()]
                    for jj in range(4):
                        j = quad * 4 + jj
                        hq = g * 8 + j
                        kc, r0 = hq // 2, (hq % 2) * 64
                        ps = pss[jj % 2]
                        c0 = (jj // 2) * 256
                        P.mm(ps[:, c0:c0 + 128], kTd[r0:r0 + 64, g, psl], qTn[r0:r0 + 64, kc, :], True, True)
                        P.mm(ps[:, c0 + 128:c0 + 256], kTd[r0:r0 + 64, g, nsl], qTn[r0:r0 + 64, kc, :], True, True)
                    for jj in range(4):
                        c0 = (jj // 2) * 256
                        P.act(pe_[:, quad * 4 + jj, :], pss[jj % 2][:, c0:c0 + 256], AF.Exp, scale=0.125)
                P.tt(pe_.v, pe_.v, m3, ALU.mult)
                pos_ = [next_pb(), next_pb()]
                for j in range(8):
                    oc = pos_[j // 4][:, (j % 4) * 65:(j % 4 + 1) * 65]
                    if n > 0:
                        P.mm(oc, pe_[:, j, 0:128], vext[n - 1][:, g, :], True, False)
                        P.mm(oc, pe_[:, j, 128:256], vext[n][:, g, :], False, True)
                    else:
                        P.mm(oc, pe_[:, j, 128:256], vext[n][:, g, :], True, True)
                for q4 in range(2):
                    po3 = pos_[q4][:, 0:260].rearrange("p (j e) -> p j e", e=65)
                    dsl = slice(g * 8 + q4 * 4, g * 8 + q4 * 4 + 4)
                    P.tt(den[:, dsl].rearrange("p (j o) -> p j o", o=1), po3[:, :, 64:65],
                         sinkrep[:, dsl].rearrange("p (j o) -> p j o", o=1), ALU.add)
                    P.recip(den[:, dsl], den[:, dsl])
                    P.tt(o_t[:, (g * 8 + q4 * 4) * 64:(g * 8 + q4 * 4 + 4) * 64].rearrange("p (j d) -> p j d", d=64),
                         po3[:, :, 0:64], den[:, dsl].rearrange("p (j o) -> p j o", o=1).bcast([128, 4, 64]), ALU.mult)
            transpose8(o_t.v, [(oT.v, evac_eng())])
            for hh in range(2):
                ps = next_pb()
                for kc in range(KC):
                    P.mm(ps.v, oT[:, kc, :], wo2[hh][:, kc, :], kc == 0, kc == KC - 1)
                P.tt(xs[n][:, hh * 512:(hh + 1) * 512], xs[n][:, hh * 512:(hh + 1) * 512], ps.v, ALU.add)

    stages = [("mod0a", lambda: layer_mod(0, 0)), ("gdn", gdn), ("mod0b", lambda: layer_mod(0, 1)), ("moe0", lambda: moe(0)),
              ("mod1a", lambda: layer_mod(1, 0)), ("attn", layer1_mixer), ("mod1b", lambda: layer_mod(1, 1)),
              ("moe1", lambda: moe(1))]
    try:
        for name, fn in stages:
            fn()
            if stop_after == name:
                break
    except _Cut:
        pass
    P.scope()
    for i in range(NT):
        P.dma("sp", y_out[i * 128:(i + 1) * 128, :], xs[i].v, key=xs[i])
    P.emit()
    return nc, P


def make_in_maps(inputs, S=2048, NE=32):
    f = lambda a: np.ascontiguousarray(np.asarray(a, dtype=np.float32))
    B = inputs["x"].shape[0]
    NT = S // 128
    up_b = np.asarray(inputs["up_b"], dtype=np.float32)
    shared = {
        "ada_w": f(inputs["ada_w"]), "ada_b": f(inputs["ada_b"]), "norm_gain": f(inputs["norm_gain"]).reshape(4, D),
        "a_w_in": f(inputs["a_w_in"][0]), "a_convT": f(np.asarray(inputs["a_conv"][0]).T), "a_log": f(inputs["a_log"][0]),
        "a_dt_bias": f(inputs["a_dt_bias"][0]), "a_out_gain": f(inputs["a_out_gain"][0]), "a_w_out": f(inputs["a_w_out"][0]),
        "kv_ada_w": f(inputs["kv_ada_w"]), "kv_ada_b": f(inputs["kv_ada_b"]).reshape(1, -1),
        "kv_norm_gain": f(inputs["kv_norm_gain"]).reshape(1, -1),
        "kv_w": f(inputs["kv_w"]), "k_norm_gain": f(inputs["k_norm_gain"]), "b_w_q": f(inputs["b_w_q"][0]),
        "q_norm_gain": f(inputs["q_norm_gain"][0]), "b_sinks": f(inputs["b_sinks"][0]), "b_w_out": f(inputs["b_w_out"][0]),
        "router_w": f(inputs["router_w"]), "router_b": f(inputs["router_b"]), "up_w": f(inputs["up_w"]),
        "up_bT": f(up_b.reshape(2, up_b.shape[1], 16, 128).transpose(0, 1, 3, 2)),
        "down_w": f(inputs["down_w"]), "down_b": f(inputs["down_b"]),
    }
    maps = []
    for b in range(B):
        m = dict(shared)
        m["x"] = f(inputs["x"][b])
        m["cT"] = f(np.asarray(inputs["c"][b]).reshape(KC, 128).T)
        m["pos"] = np.ascontiguousarray(np.asarray(inputs["positions"][b], dtype=np.int32).reshape(NT, 128).T)
        maps.append(m)
    return maps


_CACHE = {}


def kernel(**inputs):
    if "nc" not in _CACHE:
        _CACHE["nc"] = build()[0]
    nc = _CACHE["nc"]
    maps = make_in_maps(inputs)
    res = run_bass_kernel_spmd(nc, maps, core_ids=list(range(len(maps))))
    return np.stack([np.asarray(r["y"], dtype=np.float32) for r in res.results], axis=0)
```

```python
import numpy as np
from contextlib import ExitStack
import concourse.bass as bass
import concourse.mybir as mybir
from concourse.bass_utils import run_bass_kernel_spmd

F32 = mybir.dt.float32
BF16 = mybir.dt.bfloat16
I32 = mybir.dt.int32
U32 = mybir.dt.uint32
AF = mybir.ActivationFunctionType
ALU = mybir.AluOpType
ENGINES = ("pe", "act", "dve", "pool", "sp")
NEG = -60000.0


class V:
    __slots__ = ("t", "ap")

    def __init__(self, t, ap):
        self.t, self.ap = t, ap

    def __getitem__(self, k):
        return V(self.t, self.ap[k])

    def bitcast(self, dt):
        return V(self.t, self.ap.bitcast(dt))

    def rearrange(self, s, **kw):
        return V(self.t, self.ap.rearrange(s, **kw))

    def bcast(self, shape):
        return V(self.t, self.ap.to_broadcast(list(shape)))


class T:
    _n = 0

    def __init__(self, ap, name):
        self.ap0 = ap
        self.name = name
        self.id = T._n
        T._n += 1
        self.last_writer = None
        self.readers = []
        self.psum = False

    def __getitem__(self, k):
        return V(self, self.ap0[k])

    @property
    def v(self):
        return V(self, self.ap0)


class Op:
    __slots__ = ("eng", "fn", "reads", "writes", "is_dma", "key", "deps", "signal",
                 "tok_sem", "tok_val", "idx", "sid")


class Prog:
    def __init__(self, nc):
        self.nc = nc
        self.ops = []
        self.stack = ExitStack()
        self.pending = {}
        self.last_eng_op = {}
        self.last_key_op = {}
        self.arena = None
        self.arena_off = 0
        self.arena_words = 0
        self.scope_id = 0

    def make_arena(self, words):
        self.arena = self.stack.enter_context(self.nc.sbuf_tensor("arena", [128, words], F32))
        self.arena_words = words

    def scope(self):
        deps = set(self.last_eng_op.values()) | set(self.last_key_op.values())
        self.pending = {e: set(deps) for e in ENGINES}
        self.arena_off = 0
        self.scope_id += 1

    def sc(self, name, shape, dtype=F32):
        n = 1
        for d in shape[1:]:
            n *= d
        words = (n + 1) // 2 if dtype == BF16 else n
        words = (words + 7) // 8 * 8
        assert self.arena_off + words <= self.arena_words, ("arena overflow", name, self.arena_off + words)
        ap = self.arena[0:shape[0], self.arena_off:self.arena_off + words]
        self.arena_off += words
        if dtype != F32:
            ap = ap.bitcast(dtype)
        ap = ap[:, 0:n]
        if len(shape) == 3:
            ap = ap.rearrange("p (a b) -> p a b", a=shape[1])
        return T(ap, name)

    def sb(self, name, shape, dtype=F32):
        t = self.stack.enter_context(self.nc.sbuf_tensor(name, list(shape), dtype))
        return T(t[tuple(slice(None) for _ in shape)], name)

    def ps(self, name, shape, dtype=F32):
        t = self.stack.enter_context(self.nc.psum_tensor(name, list(shape), dtype))
        tt = T(t[tuple(slice(None) for _ in shape)], name)
        tt.psum = True
        return tt

    def dram(self, name, shape, dtype=F32, kind="Internal"):
        t = self.nc.dram_tensor(name, list(shape), dtype, kind=kind)
        return T(t.ap(), name)

    def _add(self, eng, fn, reads, writes, is_dma=False, key=None):
        o = Op()
        o.eng, o.fn, o.is_dma, o.key = eng, fn, is_dma, key
        o.reads = list({t.id: t for t in reads}.values())
        o.writes = list({t.id: t for t in list(writes) + [t for t in reads if t.psum]}.values())
        o.deps, o.signal = set(), False
        i = len(self.ops)
        o.idx = i
        o.sid = self.scope_id
        for t in o.reads:
            if t.last_writer is not None:
                o.deps.add(t.last_writer)
        for t in o.writes:
            if t.last_writer is not None:
                o.deps.add(t.last_writer)
            o.deps.update(t.readers)
        for t in o.reads:
            t.readers.append(i)
        for t in o.writes:
            t.last_writer = i
            t.readers = []
        if self.pending.get(eng):
            o.deps |= self.pending.pop(eng)
        if is_dma:
            self.last_key_op[key.id] = i
        else:
            self.last_eng_op[eng] = i
        o.deps.discard(i)
        self.ops.append(o)
        return o

    def I(self, eng, fn, ins, outs):
        return self._add(eng, fn, [v.t for v in ins], [v.t for v in outs])

    def dma(self, eng, out, in_, key=None, **kw):
        k = key if key is not None else (out.t if out.t.name[0] != "@" else in_.t)
        return self._add(eng, lambda e: e.dma_start(out=out.ap, in_=in_.ap, **kw), [in_.t], [out.t],
                         is_dma=True, key=k)

    def mm(self, out, lhsT, rhs, start=True, stop=True, extra_reads=()):
        return self.I("pe", lambda e: e.matmul(out.ap, lhsT=lhsT.ap, rhs=rhs.ap, start=start, stop=stop),
                      [lhsT, rhs] + ([] if start else [out]) + list(extra_reads), [out])

    def tr(self, out, in_, ident):
        return self.I("pe", lambda e: e.transpose(out=out.ap, in_=in_.ap, identity=ident.ap), [in_, ident], [out])

    def act(self, out, in_, func, bias=None, scale=None, accum=None, eng="act"):
        ins = [in_] + ([bias] if isinstance(bias, V) else []) + ([scale] if isinstance(scale, V) else [])
        outs = [out] + ([accum] if accum is not None else [])

        def fn(e):
            kw = {}
            if bias is not None:
                kw["bias"] = bias.ap if isinstance(bias, V) else bias
            if scale is not None:
                kw["scale"] = scale.ap if isinstance(scale, V) else scale
            if accum is not None:
                kw["accum_out"] = accum.ap
            return e.activation(out=out.ap, in_=in_.ap, func=func, **kw)
        return self.I(eng, fn, ins, outs)

    def ts(self, out, in0, s1, op0, s2=None, op1=None, eng="dve"):
        ins = [in0] + [s for s in (s1, s2) if isinstance(s, V)]

        def fn(e):
            a1 = s1.ap if isinstance(s1, V) else s1
            a2 = s2.ap if isinstance(s2, V) else s2
            if op1 is None:
                return e.tensor_scalar(out=out.ap, in0=in0.ap, scalar1=a1, scalar2=None, op0=op0)
            return e.tensor_scalar(out=out.ap, in0=in0.ap, scalar1=a1, scalar2=a2, op0=op0, op1=op1)
        return self.I(eng, fn, ins, [out])

    def tt(self, out, in0, in1, op, eng="dve"):
        return self.I(eng, lambda e: e.tensor_tensor(out=out.ap, in0=in0.ap, in1=in1.ap, op=op), [in0, in1], [out])

    def stt(self, out, in0, scalar, in1, op0, op1):
        ins = [in0, in1] + ([scalar] if isinstance(scalar, V) else [])
        return self.I("dve", lambda e: e.scalar_tensor_tensor(
            out=out.ap, in0=in0.ap, scalar=(scalar.ap if isinstance(scalar, V) else scalar),
            in1=in1.ap, op0=op0, op1=op1), ins, [out])

    def copy(self, out, in_, eng="dve"):
        if eng == "act":
            return self.act(out, in_, AF.Copy)
        return self.I(eng, lambda e: e.tensor_copy(out=out.ap, in_=in_.ap), [in_], [out])

    def memset(self, out, val, eng="pool"):
        return self.I(eng, lambda e: e.memset(out.ap, val), [], [out])

    def recip(self, out, in_):
        return self.I("dve", lambda e: e.reciprocal(out=out.ap, in_=in_.ap), [in_], [out])

    def affsel(self, out, in_, pattern, cmp, fill, base, cm):
        return self.I("pool", lambda e: e.affine_select(out=out.ap, in_=in_.ap, pattern=pattern, compare_op=cmp,
                                                        fill=fill, base=base, channel_multiplier=cm), [in_], [out])

    def emit(self):
        nc, ops = self.nc, self.ops
        for o in ops:
            keep = set()
            for d in o.deps:
                p = ops[d]
                if not p.is_dma and not o.is_dma and p.eng == o.eng:
                    if o.eng == "pe":
                        continue
                keep.add(d)
            o.deps = keep
            for d in keep:
                ops[d].signal = True
        eng_cnt = {e: 0 for e in ENGINES}
        key_cnt, sems = {}, {}
        slot_of, nslot = {}, {}
        for o in ops:
            if o.is_dma:
                sw = o.eng == "pool"
                sk = (o.sid, o.key.id, sw)
                if sk not in slot_of:
                    n = nslot.get((o.sid, sw), 0)
                    slot_of[sk] = (sw, n)
                    nslot[(o.sid, sw)] = n + 1
                k = slot_of[sk]
                key_cnt[k] = key_cnt.get(k, 0) + 16
                o.tok_sem, o.tok_val = ("k", k), key_cnt[k]
            elif o.signal:
                eng_cnt[o.eng] += 1
                o.tok_sem, o.tok_val = ("e", o.eng), eng_cnt[o.eng]
        for e in ENGINES:
            sems[("e", e)] = self.stack.enter_context(nc.semaphore("sem_" + e))
        for k in key_cnt:
            sems[("k", k)] = self.stack.enter_context(nc.semaphore("semk_%d_%d" % (int(k[0]), k[1])))
        self.n_sems = len(sems)
        final = [(("k", k), v) for k, v in key_cnt.items()] + [(("e", e), v) for e, v in eng_cnt.items() if v > 0]
        with nc.Block() as block:
            def mk(en):
                def body(eng):
                    waited = {}
                    for o in ops:
                        if o.eng != en:
                            continue
                        need = {}
                        for d in o.deps:
                            p = ops[d]
                            if p.tok_val > need.get(p.tok_sem, 0):
                                need[p.tok_sem] = p.tok_val
                        for s, v in need.items():
                            if waited.get(s, 0) >= v:
                                continue
                            eng.wait_ge(sems[s], v)
                            waited[s] = v
                        ins = o.fn(eng)
                        if o.is_dma:
                            ins.then_inc(sems[o.tok_sem], 16)
                        elif o.signal:
                            ins.then_inc(sems[o.tok_sem], 1)
                    if en == "sp":
                        for s, v in final:
                            if waited.get(s, 0) < v:
                                eng.wait_ge(sems[s], v)
                return body
            block.tensor(mk("pe"))
            block.scalar(mk("act"))
            block.vector(mk("dve"))
            block.gpsimd(mk("pool"))
            block.sync(mk("sp"))
        self.stack.close()


D = 1024
KC = 8
EPS = 1e-6
ARENA_WORDS = 19840


class _Cut(Exception):
    pass


def build(S=2048, NE=32, stop_after=None, cut=None):
    NT = S // 128
    TQ = min(S, 512)
    NQ = S // TQ
    CPQ = TQ // 128
    nc = bass.Bass("TRN2", target_bir_lowering=False)
    P = Prog(nc)

    def din(name, shape, dtype=F32):
        t = P.dram(name, shape, dtype, kind="ExternalInput")
        t.name = "@" + name
        return t

    x_in = din("x", [S, D]); cT_in = din("cT", [128, KC]); pos_in = din("pos", [128, NT], I32)
    ada_w = din("ada_w", [2, D, 6 * D]); ada_b = din("ada_b", [2, 6 * D]); norm_gain = din("norm_gain", [4, D])
    a_w_in = din("a_w_in", [D, 4112]); a_convT = din("a_convT", [3072, 4]); a_log = din("a_log", [8])
    a_dt_bias = din("a_dt_bias", [8]); a_out_gain = din("a_out_gain", [128]); a_w_out = din("a_w_out", [D, D])
    kv_ada_w = din("kv_ada_w", [D, 2 * D]); kv_ada_b = din("kv_ada_b", [1, 2 * D]); kv_norm_gain = din("kv_norm_gain", [1, D])
    kv_w = din("kv_w", [D, 256]); k_norm_gain = din("k_norm_gain", [64]); b_w_q = din("b_w_q", [D, D])
    q_norm_gain = din("q_norm_gain", [64]); b_sinks = din("b_sinks", [16]); b_w_out = din("b_w_out", [D, D])
    router_w = din("router_w", [2, D, NE]); router_b = din("router_b", [2, NE])
    up_w = din("up_w", [2, NE, D, 2 * D]); up_bT = din("up_bT", [2, NE, 128, 16])
    down_w = din("down_w", [2, NE, D, D]); down_b = din("down_b", [2, NE, D])
    y_out = P.dram("y", [S, D], F32, kind="ExternalOutput"); y_out.name = "@y"

    def ck(n):
        if cut is not None and cut == n:
            raise _Cut()

    def wview(v):
        v = v.v if isinstance(v, T) else v
        return v.rearrange("(kc p) f -> p kc f", p=128)

    def pbc(t, idx=None):
        ap = t.ap0 if idx is None else t.ap0[idx]
        return V(t, ap.partition_broadcast(128))

    xs = [P.sb("x%d" % i, [128, D]) for i in range(NT)]
    hT = P.sb("hT", [128, KC, S], BF16)
    ident = P.sb("ident", [128, 128]); ones = P.sb("ones", [128, 128])
    Uincl = P.sb("Uincl", [128, 128])
    MA = P.sb("MA", [128, 128]); zeros = P.sb("zeros", [128, 128])
    m01 = P.sb("m01", [128, 256], BF16)
    modt = [P.sb("mod%d" % j, [128, D]) for j in range(3)]
    crep = P.sb("crep", [128, KC, 128], BF16)
    cact = P.sb("cact", [128, KC])
    rowb = P.sb("rowb", [1, 512]); rowg = P.sb("rowg", [1, 512])
    NPC = 4
    piece = []
    pc_i = [0]

    def alloc_pieces():
        piece[:] = [P.sc("piece%d" % i, [128, KC, 512], BF16) for i in range(NPC)]
        pc_i[0] = 0
    hrow = P.sb("hrow", [128, D]); htmp = P.sb("htmp", [128, D]); ssq = P.sb("ssq", [128, 4])
    gates = [P.sb("gates%d" % i, [128, NE]) for i in range(NT)]
    gks = [P.sb("gks%d" % i, [128, 4]) for i in range(NT)]
    idxs = [P.sb("idxs%d" % i, [128, 4], I32) for i in range(NT)]
    zeros_row = P.sb("zeros_row", [1, 512])
    xg = P.dram("xg", [NE * (S // 2) + 1, D], BF16); xg.name = "@xg"
    yg = P.dram("yg", [NE * (S // 2) + 1, D], F32); yg.name = "@yg"
    P.make_arena(ARENA_WORDS)

    def next_piece():
        p = piece[pc_i[0] % NPC]
        pc_i[0] += 1
        return p
    pb = [P.ps("pb%d" % i, [128, 512]) for i in range(8)]
    pb_i = [0]

    def next_pb():
        p = pb[pb_i[0] % 8]
        pb_i[0] += 1
        return p
    ev_i = [0]

    def evac_eng():
        ev_i[0] += 1
        return "act" if ev_i[0] % 2 else "dve"

    P.memset(ones.v, 1.0); P.memset(zeros.v, 0.0); P.memset(zeros_row.v, 0.0)
    P.affsel(ident.v, ones.v, [[1, 128]], ALU.is_equal, 0.0, 0, -1)
    P.affsel(Uincl.v, ones.v, [[1, 128]], ALU.is_ge, 0.0, 0, -1)
    P.affsel(MA.v, zeros.v, [[-1, 128]], ALU.is_gt, NEG, 0, 1)
    P.affsel(hrow[:, 0:128], ones.v, [[-1, 128]], ALU.is_gt, 0.0, 0, 1)
    P.copy(m01[:, 0:128], hrow[:, 0:128], eng="pool")
    P.copy(m01[:, 128:256], Uincl.v, eng="pool")

    for i in range(NT):
        P.dma("sp", xs[i].v, x_in[i * 128:(i + 1) * 128, :])
    P.dma("sp", cact.v, cT_in.v)
    P.act(cact.v, cact.v, AF.Silu)
    for kc in range(KC):
        P.ts(crep[:, kc, :], ones.v, cact[:, kc:kc + 1], ALU.mult)

    def compute_mod(wdram, brow, col0, outs):
        for n, o in enumerate(outs):
            c0 = col0 + n * 512
            pc = next_piece()
            P.dma("pool", pc.v, wview(wdram)[:, :, c0:c0 + 512])
            P.dma("sp", rowb.v, brow[0:1, c0:c0 + 512])
            ps = next_pb()
            P.mm(ps.v, ones[0:1, :], rowb.v, True, False)
            for kc in range(KC):
                P.mm(ps.v, crep[:, kc, :], pc[:, kc, :], False, kc == KC - 1)
            P.copy(o, ps.v, eng=evac_eng())

    def gain_fold(dst, grow):
        for hh in range(2):
            P.dma("sp", rowg.v, grow[0:1, hh * 512:(hh + 1) * 512])
            ps = next_pb()
            P.mm(ps.v, ones[0:1, :], rowg.v, True, True)
            P.stt(dst[:, hh * 512:(hh + 1) * 512], dst[:, hh * 512:(hh + 1) * 512], 1.0, ps.v, ALU.add, ALU.mult)

    def layer_mod(l, half):
        P.scope()
        alloc_pieces()
        outs = []
        for j in range(3):
            outs += [modt[j][:, 0:512], modt[j][:, 512:1024]]
        compute_mod(ada_w[l], ada_b[l:l + 1, :], half * 3 * D, outs)
        gain_fold(modt[1], norm_gain[2 * l + half:2 * l + half + 1, :])

    def rms_rstd(dst, src_sum, n):
        P.ts(dst, src_sum, 1.0 / n, ALU.mult, EPS, ALU.add)
        P.act(dst, dst, AF.Sqrt)
        P.recip(dst, dst)

    def norm_mod_tile(i, A, B, out_f32):
        P.act(htmp.v, xs[i].v, AF.Square, accum=ssq[:, 0:1])
        rms_rstd(ssq[:, 1:2], ssq[:, 0:1], D)
        P.stt(htmp.v, xs[i].v, ssq[:, 1:2], A.v, ALU.mult, ALU.mult)
        P.tt(out_f32, htmp.v, B.v, ALU.add, eng="pool")

    def transpose8(src_f32, dsts):
        for half in range(2):
            ps = next_pb()
            for q in range(4):
                kc = half * 4 + q
                P.tr(ps[:, q * 128:(q + 1) * 128], src_f32[:, kc * 128:(kc + 1) * 128], ident.v)
            for dv, eng in dsts:
                P.copy(dv[:, half * 4:half * 4 + 4, :], ps.v.rearrange("p (q t) -> p q t", q=4), eng=eng)

    def norm_all(A, B):
        for i in range(NT):
            norm_mod_tile(i, A, B, hrow.v)
            transpose8(hrow.v, [(hT[:, :, i * 128:(i + 1) * 128], evac_eng())])

    bc_cache = {}

    def bc_reg(e, val):
        if val not in bc_cache:
            bc_cache[val] = e.to_reg(val)
        return bc_cache[val]

    def moe(l):
        B, A, G = modt
        CAP = S // 2
        NB = CAP // 128
        NH = max(CAP // 512, 1)
        HW_ = min(CAP, 512)
        TRASH = NE * CAP
        P.scope()
        h2T_f = P.sc("h2Tf", [128, KC, 128]); rw_sb = P.sc("rw_sb", [128, KC, NE]); rbrep = P.sc("rbrep", [128, NE])
        lg = P.sc("lg", [128, NE]); lgr = P.sc("lgr", [128, NE]); top8 = P.sc("top8", [128, 8]); msk = P.sc("msk", [128, NE])
        sm = P.sc("sm", [128, 4]); idx8 = P.sc("idx8", [128, 8], U32); ef = P.sc("ef", [128, 4])
        db_sb = P.sc("db_sb", [NE, D]); gT = P.sc("gT", [NE, 128]); gpad = P.sc("gpad", [128, 128])
        runsum = P.sc("runsum", [128, NE]); pos = P.sc("pos", [128, NE]); ov = P.sc("ov", [128, NE]); t1 = P.sc("t1", [128, NE])
        iota_e = P.sc("iota_e", [128, NE]); base_e = P.sc("base_e", [128, NE]); iota_i = P.sc("iota_i", [128, NE], I32)
        oh = P.sc("oh", [128, NE]); ohj = P.sc("ohj", [128, NE]); destf = P.sc("destf", [128, 4])
        Ustr = P.sc("Ustr", [128, 128])
        h2b = [P.sc("h2b%d" % j, [128, D], BF16) for j in range(2)]
        zt = P.sc("zt", [128, 4096], BF16)
        P.memset(gpad.v, 0.0)
        P.memset(runsum.v, 0.0)
        P.tt(Ustr.v, Uincl.v, ident.v, ALU.subtract, eng="pool")
        for e_ in range(NE):
            P.memset(base_e[:, e_:e_ + 1], float(e_ * CAP), eng="pool")
        if l == 0:
            P.memset(zt.v, 0.0, eng="pool")
            rows_per = 128 * 4
            for r0 in range(0, NE * CAP, rows_per):
                P.dma("sp", xg[r0:r0 + rows_per, :].rearrange("(p a) d -> p (a d)", p=128), zt.v, key=zt)
            P.dma("sp", xg[TRASH:TRASH + 1, :], zt[0:1, 0:D], key=zt)
            P.copy(lg[0:1, 0:NE], zt[0:1, 0:NE])
            for q in range(2):
                P.dma("sp", yg[TRASH:TRASH + 1, q * 512:(q + 1) * 512], zeros_row.v, key=zeros_row)
        P.dma("sp", rw_sb.v, wview(router_w[l]))
        P.dma("sp", rbrep.v, pbc(router_b, l))
        P.dma("sp", db_sb.v, down_b[l])
        P.tt(db_sb.v, db_sb.v, G[0:NE, :], ALU.mult, eng="pool")
        for i in range(NT):
            norm_mod_tile(i, A, B, hrow.v)
            hb = h2b[i % 2]
            P.copy(hb.v, hrow.v, eng="pool")
            transpose8(hrow.v, [(h2T_f.v, evac_eng())])
            ps = next_pb()
            for kc in range(KC):
                P.mm(ps[:, 0:NE], h2T_f[:, kc, :], rw_sb[:, kc, :], kc == 0, kc == KC - 1)
            P.tt(lgr.v, ps[:, 0:NE], rbrep.v, ALU.add)
            P.I("dve", lambda e: e.max(out=top8.v.ap, in_=lgr.v.ap), [lgr.v], [top8.v])
            P.ts(msk.v, lgr.v, top8[:, 3:4], ALU.is_ge)
            P.ts(sm[:, 0:1], top8[:, 0:1], -1.0, ALU.mult)
            P.act(lg.v, lgr.v, AF.Exp, bias=sm[:, 0:1], scale=1.0)
            P.tt(lg.v, lg.v, msk.v, ALU.mult)
            P.I("dve", lambda e: e.reduce_sum(out=sm[:, 1:2].ap, in_=lg.v.ap, axis=mybir.AxisListType.X),
                [lg.v], [sm[:, 1:2]])
            P.recip(sm[:, 2:3], sm[:, 1:2])
            P.ts(gpad[:, 0:NE], lg.v, sm[:, 2:3], ALU.mult)
            P.copy(gates[i].v, gpad[:, 0:NE], eng="pool")
            P.act(gks[i].v, top8[:, 0:4], AF.Exp, bias=sm[:, 0:1], scale=1.0)
            P.ts(gks[i].v, gks[i].v, sm[:, 2:3], ALU.mult)
            ps = next_pb()
            P.tr(ps[:, 0:128], gpad.v, ident.v)
            P.copy(gT.v, ps[0:NE, 0:128], eng="act")
            for hh in range(2):
                ps = next_pb()
                P.mm(ps.v, gT.v, db_sb[:, hh * 512:(hh + 1) * 512], True, True)
                P.tt(xs[i][:, hh * 512:(hh + 1) * 512], xs[i][:, hh * 512:(hh + 1) * 512], ps.v, ALU.add)
            ps = next_pb()
            P.mm(ps[:, 0:NE], Ustr.v, msk.v, True, False)
            P.mm(ps[:, 0:NE], ones.v, runsum.v, False, True)
            P.copy(pos.v, ps[:, 0:NE], eng="act")
            P.tt(runsum.v, runsum.v, msk.v, ALU.add, eng="pool")
            P.ts(ov.v, pos.v, float(CAP), ALU.is_ge)
            P.tt(pos.v, pos.v, base_e.v, ALU.add)
            P.tt(t1.v, pos.v, ov.v, ALU.mult)
            P.tt(pos.v, pos.v, t1.v, ALU.subtract)
            P.stt(pos.v, ov.v, float(TRASH), pos.v, ALU.mult, ALU.add)
            for k in range(4):
                P.ts(oh.v, lgr.v, top8[:, k:k + 1], ALU.is_equal)
                P.tt(ohj.v, oh.v, pos.v, ALU.mult)
                P.I("dve", lambda e, k=k: e.reduce_sum(out=destf[:, k:k + 1].ap, in_=ohj.v.ap, axis=mybir.AxisListType.X),
                    [ohj.v], [destf.v])
            P.copy(idxs[i].v, destf.v)
            for k in range(4):
                P._add("pool", (lambda e, k=k, hb=hb, ix=idxs[i]: e.indirect_dma_start(
                    out=xg.ap0[:, :], out_offset=bass.IndirectOffsetOnAxis(ap=ix[:, k:k + 1].ap, axis=0),
                    in_=hb.v.ap, in_offset=None, bounds_check=bc_reg(e, TRASH), oob_is_err=False)),
                    [hb, idxs[i]], [xg], is_dma=True, key=hb)
        P.scope()
        alloc_pieces()
        xT = T(hT.ap0[:, :, 0:CAP], "xT"); actT = T(hT.ap0[:, :, CAP:2 * CAP], "actT")
        xin = [P.sc("xin%d" % j, [128, D], BF16) for j in range(NB)]

        def load_x(e_):
            for b_ in range(NB):
                P.dma("sp", xin[b_].v, xg[e_ * CAP + b_ * 128: e_ * CAP + (b_ + 1) * 128, :])
        ub = P.sc("ub", [128, 16]); ub1 = P.sc("ub1", [128, 8])
        gact = [P.sc("gact%d" % j, [128, HW_]) for j in range(2)]
        sgm = [P.sc("sgm%d" % j, [128, HW_], BF16) for j in range(2)]
        gsb = [P.sc("gsb%d" % j, [128, HW_], BF16) for j in range(2)]
        lin1 = [P.sc("lin1%d" % j, [128, HW_]) for j in range(2)]
        ybuf = [P.sc("ybuf%d" % j, [128, D]) for j in range(2)]
        identb = P.sc("identb", [128, 128], BF16)
        P.copy(identb.v, ident.v, eng="pool")
        cnt = 0
        load_x(0)
        for e in range(NE):
            P.dma("sp", ub.v, up_bT[l, e])
            P.ts(ub1.v, ub[:, 8:16], 1.0, ALU.add)
            for b in range(NB):
                xi = xin[b]
                ps = next_pb()
                psb = ps.v.bitcast(BF16)
                for kc in range(KC):
                    P.tr(psb[:, kc * 128:(kc + 1) * 128], xi[:, kc * 128:(kc + 1) * 128], identb.v)
                P.copy(xT[:, :, b * 128:(b + 1) * 128], psb.rearrange("p (k t) -> p k t", k=KC), eng=evac_eng())
            if e + 1 < NE:
                load_x(e + 1)
            pds = []
            for quad in range(2):
                pg, pl = next_piece(), next_piece()
                P.dma("pool", pg.v, wview(up_w[l, e])[:, :, quad * 512:(quad + 1) * 512])
                P.dma("pool", pl.v, wview(up_w[l, e])[:, :, 1024 + quad * 512:1024 + (quad + 1) * 512])
                for mm_ in range(4):
                    m = quad * 4 + mm_
                    for hf in range(NH):
                        ssl = slice(hf * HW_, (hf + 1) * HW_)
                        bb = cnt % 2
                        cnt += 1
                        psg, psl = next_pb(), next_pb()
                        for kc in range(KC):
                            P.mm(psg[:, 0:HW_], pg[:, kc, mm_ * 128:(mm_ + 1) * 128], xT[:, kc, ssl], kc == 0, kc == KC - 1)
                        for kc in range(KC):
                            P.mm(psl[:, 0:HW_], pl[:, kc, mm_ * 128:(mm_ + 1) * 128], xT[:, kc, ssl], kc == 0, kc == KC - 1)
                        P.ts(gact[bb].v, psg[:, 0:HW_], ub[:, m:m + 1], ALU.add, 7.0, ALU.min)
                        P.act(sgm[bb].v, gact[bb].v, AF.Sigmoid, scale=1.702)
                        P.tt(gsb[bb].v, gact[bb].v, sgm[bb].v, ALU.mult)
                        P.ts(lin1[bb].v, psl[:, 0:HW_], ub1[:, m:m + 1], ALU.add, 8.0, ALU.min)
                        P.stt(actT[:, m, ssl], lin1[bb].v, -6.0, gsb[bb].v, ALU.max, ALU.mult)
            for quad in range(2):
                pd = next_piece()
                dsrc = down_w[l, e][quad * 512:(quad + 1) * 512, :].rearrange("(c p) f -> p c f", p=128)
                pdv = pd.v.rearrange("p a b -> p (a b)").rearrange("p (c f) -> p c f", c=4)
                P.dma("pool", pdv, dsrc)
                pds.append(pdv)
            for b in range(NB):
                yb = ybuf[b % 2]
                for hh in range(2):
                    ps = next_pb()
                    for m in range(8):
                        P.mm(ps.v, actT[:, m, b * 128:(b + 1) * 128], pds[m // 4][:, m % 4, hh * 512:(hh + 1) * 512],
                             m == 0, m == 7)
                    P.copy(yb[:, hh * 512:(hh + 1) * 512], ps.v, eng=evac_eng())
                P.dma("sp", yg[e * CAP + b * 128: e * CAP + (b + 1) * 128, :], yb.v, key=yb)
        P.scope()
        yks = [[P.sc("yk%d_%d" % (r, j), [128, D]) for j in range(4)] for r in range(3)]
        accs = [P.sc("acc%d" % r, [128, D]) for r in range(2)]
        for i in range(NT):
            yk = yks[i % 3]
            for k in range(4):
                P._add("pool", (lambda e, k=k, ix=idxs[i], yk=yk: e.indirect_dma_start(
                    out=yk[k].v.ap, out_offset=None, in_=yg.ap0[:, :],
                    in_offset=bass.IndirectOffsetOnAxis(ap=ix[:, k:k + 1].ap, axis=0), bounds_check=bc_reg(e, TRASH), oob_is_err=False)),
                    [yg, idxs[i]], [yk[k]], is_dma=True, key=yk[k])
            acc = accs[i % 2]
            P.ts(acc.v, yk[0].v, gks[i][:, 0:1], ALU.mult)
            for k in range(1, 4):
                P.stt(acc.v, yk[k].v, gks[i][:, k:k + 1], acc.v, ALU.mult, ALU.add)
            P.tt(acc.v, acc.v, G.v, ALU.mult, eng="pool")
            P.tt(xs[i].v, xs[i].v, acc.v, ALU.add)

    H = 8
    NCH = NT
    LNSC = float(np.log(128.0 ** -0.5))

    def gdn():
        B, A, G = modt
        norm_all(A, B)
        ck(1)
        P.scope()
        sq = lambda n: P.sc(n, [128, 128])
        ab_sb = P.sc("ab_sb", [128, KC, 16], BF16)
        sc_a = P.sc("sc_a", [128, NCH, 8]); sc_beta = P.sc("sc_beta", [128, NCH, 8]); sc_nbeta = P.sc("sc_nbeta", [128, NCH, 8])
        sc_g = P.sc("sc_g", [128, NCH, 8]); sc_gc = P.sc("sc_gc", [128, NCH, 8]); sc_egc = P.sc("sc_egc", [128, NCH, 8])
        sc_etail = P.sc("sc_etail", [128, NCH, 8]); sc_gtot = P.sc("sc_gtot", [128, NCH, 8]); sc_ng = P.sc("sc_ng", [128, NCH, 8])
        dtb = P.sc("dtb", [128, 8]); alog = P.sc("alog", [128, 8]); gainrep = sq("gainrep")
        TQG = 128; NQG = S // TQG; CPQG = TQG // 128

        def mkbufs(j):
            n = lambda s_: "%s_%d" % (s_, j)
            sqj = lambda s_: P.sc(n(s_), [128, 128])
            return ([P.sc(n("wqh%d" % q), [128, KC, 128], BF16) for q in range(4)], P.sc(n("wout_h"), [128, D], BF16),
                    P.sc(n("convw"), [128, 3, 4]), [P.sc(n("pre%d" % q), [128, TQG + 3]) for q in range(3)],
                    P.sc(n("cvt"), [128, TQG]), [P.sc(n("qkv%d" % q), [128, TQG]) for q in range(3)],
                    sqj("sqb"), P.sc(n("rn"), [128, 8]), P.sc(n("fac"), [128, 8]),
                    sqj("gU"), sqj("gUq"), sqj("gUk"), sqj("Elow"), sqj("Eincl"), sqj("egq"),
                    sqj("Nm0"), sqj("NmT0"), sqj("PTm0"),
                    sqj("QKm"), sqj("QKmT"), sqj("qdT"), sqj("vb"), sqj("kbg"), sqj("ktail"),
                    sqj("Sst"), sqj("o_sb"), sqj("z_sb"), sqj("og"),
                    P.sc(n("ogT"), [128, 128], BF16))
        KH = 3
        bufs = [mkbufs(j) for j in range(KH)]
        P.dma("pool", ab_sb.v, wview(a_w_in)[:, :, 4096:4112])
        P.dma("sp", dtb.v, pbc(a_dt_bias)); P.dma("sp", alog.v, pbc(a_log)); P.dma("sp", gainrep.v, pbc(a_out_gain))
        P.act(alog.v, alog.v, AF.Exp)
        ck(2)
        for c in range(NCH):
            ps = next_pb()
            for kc in range(KC):
                P.mm(ps[:, 0:16], hT[:, kc, c * 128:(c + 1) * 128], ab_sb[:, kc, :], kc == 0, kc == KC - 1)
            P.tt(sc_a[:, c, :], ps[:, 0:8], dtb.v, ALU.add)
            P.act(sc_beta[:, c, :], ps[:, 8:16], AF.Sigmoid)
        allc = lambda t: t.v.rearrange("p c h -> p (c h)")
        P.act(allc(sc_a), allc(sc_a), AF.Exp)
        P.act(allc(sc_a), allc(sc_a), AF.Ln, bias=1.0, scale=1.0)
        for c in range(NCH):
            P.stt(sc_g[:, c, :], sc_a[:, c, :], -1.0, alog.v, ALU.mult, ALU.mult)
        P.ts(allc(sc_ng), allc(sc_g), -1.0, ALU.mult)
        P.ts(allc(sc_nbeta), allc(sc_beta), -1.0, ALU.mult)
        for c in range(NCH):
            ps = next_pb()
            P.mm(ps[:, 0:8], Uincl.v, sc_g[:, c, :], True, True)
            P.mm(ps[:, 8:16], ones.v, sc_g[:, c, :], True, True)
            P.copy(sc_gc[:, c, :], ps[:, 0:8], eng="dve")
            P.act(sc_egc[:, c, :], ps[:, 0:8], AF.Exp)
            P.act(sc_gtot[:, c, :], ps[:, 8:16], AF.Exp)
            P.tt(sc_etail[:, c, :], ps[:, 8:16], sc_gc[:, c, :], ALU.subtract)
        P.act(allc(sc_etail), allc(sc_etail), AF.Exp)
        ck(3)
        def head_gen(h, b):
            (wq_h, wout_h, convw, pre, cvt, qkv, sqb, rn, fac, gU, gUq, gUk, Elow, Eincl, egq, Nm0, NmT0, PTm0,
             QKm, QKmT, qdT, vb, kbg, ktail, Sst, o_sb, z_sb, og, ogT) = b
            Nm = [Nm0, gU]; NmT = [NmT0, gUq]; PTm = [PTm0, gUk]
            u_sb, wT, vnew = Elow, Eincl, egq
            wout_f = wout_h
            for j in range(4):
                P.dma("pool", wq_h[j].v, wview(a_w_in)[:, :, j * 1024 + h * 128: j * 1024 + (h + 1) * 128])
            P.dma("sp", convw.v, V(a_convT, a_convT.ap0.rearrange("(j h p) k -> h p j k", j=3, p=128)[h]))
            P.dma("pool", wout_h.v, a_w_out[h * 128:(h + 1) * 128, :])
            P.tt(wout_f.v, wout_h.v, G.v, ALU.mult, eng="pool")
            P.memset(Sst.v, 0.0, eng="pool")
            ck(4)
            for tq in range(NQG):
                for j in range(3):
                    if tq == 0:
                        P.memset(pre[j][:, 0:3], 0.0, eng="pool")
                    else:
                        P.copy(pre[j][:, 0:3], pre[j][:, TQG:TQG + 3], eng="pool")
                    ps = next_pb()
                    for kc in range(KC):
                        P.mm(ps[:, 0:TQG], wq_h[j][:, kc, :], hT[:, kc, tq * TQG:(tq + 1) * TQG], kc == 0, kc == KC - 1)
                    P.copy(pre[j][:, 3:3 + TQG], ps[:, 0:TQG], eng="act")
                    P.ts(cvt.v, pre[j][:, 0:TQG], convw[:, j, 0:1], ALU.mult)
                    for k in range(1, 4):
                        P.stt(cvt.v, pre[j][:, k:k + TQG], convw[:, j, k:k + 1], cvt.v, ALU.mult, ALU.add)
                    P.act(qkv[j].v, cvt.v, AF.Silu)
                    yield
                ck(5)
                for cc in range(CPQG):
                    c = tq * CPQG + cc
                    lsl = slice(cc * 128, (cc + 1) * 128)
                    csl = slice(c * 128, (c + 1) * 128)
                    qc, kcn, vc = qkv[0][:, lsl], qkv[1][:, lsl], qkv[2][:, lsl]
                    bcol = lambda t: t[:, c, h:h + 1]
                    ps = next_pb()
                    P.act(sqb.v, qc, AF.Square)
                    P.mm(ps[:, 0:1], sqb.v, ones[:, 0:1], True, True)
                    P.act(sqb.v, kcn, AF.Square)
                    P.mm(ps[:, 1:2], sqb.v, ones[:, 0:1], True, True)
                    P.ts(rn[:, 0:2], ps[:, 0:2], EPS, ALU.add)
                    yield
                    P.act(rn[:, 4:6], rn[:, 0:2], AF.Ln)
                    P.ts(rn[:, 4:5], rn[:, 4:5], -0.5, ALU.mult, LNSC, ALU.add)
                    P.ts(rn[:, 5:6], rn[:, 5:6], -0.5, ALU.mult)
                    P.act(rn[:, 2:4], rn[:, 4:6], AF.Exp)
                    ck(6)
                    P.tt(fac[:, 0:1], bcol(sc_nbeta), rn[:, 3:4], ALU.mult)
                    P.tt(fac[:, 1:2], bcol(sc_beta), bcol(sc_egc), ALU.mult)
                    P.tt(fac[:, 1:2], fac[:, 1:2], rn[:, 3:4], ALU.mult)
                    P.tt(fac[:, 2:3], bcol(sc_etail), rn[:, 3:4], ALU.mult)
                    yield
                    P.ts(gU.v, Uincl.v, bcol(sc_ng), ALU.mult)
                    P.stt(gUk.v, ident.v, rn[:, 5:6], gU.v, ALU.mult, ALU.add)
                    P.ts(gUq.v, Uincl.v, bcol(sc_g), ALU.mult)
                    P.stt(gUq.v, ident.v, rn[:, 4:5], gUq.v, ALU.mult, ALU.add)
                    yield
                    p1 = next_pb()
                    P.mm(p1[:, 0:128], ones.v, gUk.v, True, False)
                    P.mm(p1[:, 0:128], ident.v, MA.v, False, True)
                    P.mm(p1[:, 128:256], ones.v, gUq.v, True, True)
                    yield
                    P.act(Elow.v, p1[:, 0:128], AF.Exp, bias=bcol(sc_gc), scale=1.0)
                    P.act(egq.v, p1[:, 128:256], AF.Exp)
                    P.tt(qdT.v, qc, egq.v, ALU.mult)
                    P.stt(Eincl.v, ident.v, rn[:, 3:4], Elow.v, ALU.mult, ALU.add)
                    yield
                    ck(7)
                    p2 = next_pb()
                    P.mm(p2[:, 0:128], kcn, kcn, True, True)
                    P.mm(p2[:, 128:256], qc, kcn, True, True)
                    yield
                    P.stt(Nm[0].v, p2[:, 0:128], fac[:, 0:1], Elow.v, ALU.mult, ALU.mult)
                    P.stt(QKm.v, p2[:, 128:256], rn[:, 2:3], Eincl.v, ALU.mult, ALU.mult)
                    yield
                    ck(71)
                    p3 = next_pb()
                    P.tr(p3[:, 0:128], Nm[0].v, ident.v)
                    P.tr(p3[:, 128:256], QKm.v, ident.v)
                    P.tr(p3[:, 256:384], kcn, ident.v)
                    P.tr(p3[:, 384:512], vc, ident.v)
                    yield
                    ck(72)
                    P.copy(NmT[0].v, p3[:, 0:128], eng="act")
                    P.copy(QKmT.v, p3[:, 128:256], eng="act")
                    P.tt(PTm[0].v, p3[:, 0:128], ident.v, ALU.add)
                    ck(73)
                    P.ts(kbg.v, p3[:, 256:384], fac[:, 1:2], ALU.mult)
                    P.ts(ktail.v, p3[:, 256:384], fac[:, 2:3], ALU.mult)
                    P.ts(vb.v, p3[:, 384:512], bcol(sc_beta), ALU.mult)
                    yield
                    ck(8)
                    cur = 0
                    for lev in range(6):
                        nxt = 1 - cur
                        p4 = next_pb()
                        P.mm(p4[:, 0:128], NmT[cur].v, Nm[cur].v, True, True)
                        if lev < 5:
                            P.mm(p4[:, 128:256], Nm[cur].v, NmT[cur].v, True, True)
                        yield
                        P.copy(Nm[nxt].v, p4[:, 0:128], eng="act")
                        if lev < 5:
                            P.copy(NmT[nxt].v, p4[:, 128:256], eng="dve")
                        yield
                        p5 = next_pb()
                        P.mm(p5[:, 0:128], Nm[nxt].v, PTm[cur].v, True, True)
                        yield
                        P.tt(PTm[nxt].v, p5[:, 0:128], PTm[cur].v, ALU.add)
                        yield
                        cur = nxt
                    ck(9)
                    PT = PTm[cur]
                    p6 = next_pb()
                    P.mm(p6[:, 0:128], PT.v, vb.v, True, True)
                    P.mm(p6[:, 128:256], kbg.v, PT.v, True, True)
                    yield
                    P.copy(u_sb.v, p6[:, 0:128], eng="act")
                    P.copy(wT.v, p6[:, 128:256], eng="dve")
                    yield
                    p7 = next_pb()
                    P.mm(p7[:, 0:128], wT.v, Sst.v, True, True)
                    yield
                    P.tt(vnew.v, u_sb.v, p7[:, 0:128], ALU.subtract)
                    yield
                    P.mm(p7[:, 128:256], qdT.v, Sst.v, True, False)
                    P.mm(p7[:, 128:256], QKmT.v, vnew.v, False, True)
                    P.mm(p7[:, 256:384], ktail.v, vnew.v, True, True)
                    yield
                    P.copy(o_sb.v, p7[:, 128:256], eng="act")
                    P.stt(Sst.v, Sst.v, bcol(sc_gtot), p7[:, 256:384], ALU.mult, ALU.add)
                    ck(10)
                    p8 = next_pb()
                    for kc in range(KC):
                        P.mm(p8[:, 0:128], hT[:, kc, csl], wq_h[3][:, kc, :], kc == 0, kc == KC - 1)
                    P.act(z_sb.v, p8[:, 0:128], AF.Silu)
                    yield
                    P.act(sqb.v, o_sb.v, AF.Square, accum=rn[:, 6:7])
                    rms_rstd(rn[:, 7:8], rn[:, 6:7], 128)
                    P.stt(og.v, o_sb.v, rn[:, 7:8], gainrep.v, ALU.mult, ALU.mult)
                    P.tt(og.v, og.v, z_sb.v, ALU.mult, eng="pool")
                    yield
                    P.tr(p8[:, 128:256], og.v, ident.v)
                    P.copy(ogT.v, p8[:, 128:256], eng="act")
                    yield
                    for hh in range(2):
                        ps = next_pb()
                        P.mm(ps.v, ogT.v, wout_f[:, hh * 512:(hh + 1) * 512], True, True)
                        P.tt(xs[c][:, hh * 512:(hh + 1) * 512], xs[c][:, hh * 512:(hh + 1) * 512], ps.v, ALU.add)

        for h0 in range(0, H, KH):
            gens = [head_gen(h0 + j, bufs[j]) for j in range(KH) if h0 + j < H]
            while gens:
                for g_ in list(gens):
                    try:
                        next(g_)
                    except StopIteration:
                        gens.remove(g_)

    TWO_PI = float(2 * np.pi)
    PI = float(np.pi)

    def layer1_mixer():
        B, A, G = modt
        P.scope()
        alloc_pieces()
        kTd = P.sc("kTd", [128, 2, S], BF16)
        vext = [P.sc("vext%d" % i, [128, 2, 65], BF16) for i in range(NT)]
        kgain = P.sc("kgain", [128, 64]); qgain = P.sc("qgain", [128, 64]); sinkrep = P.sc("sinkrep", [128, 16])
        posf = P.sc("posf", [128, NT]); posi = P.sc("posi", [128, NT], I32)
        cosb = P.sc("cosb", [128, NT, 8]); sinb = P.sc("sinb", [128, NT, 8])
        ang = P.sc("ang", [128, NT, 8]); angk = P.sc("angk", [128, NT, 8]); angi = P.sc("angi", [128, NT, 8], I32)
        invf = P.sc("invf", [128, NT, 8])
        kvw = P.sc("kvw", [128, KC, 256], BF16)
        kdup = P.sc("kdup", [128, 2, 128])
        kvt = P.sc("kvt", [128, 256]); qt = P.sc("qt", [128, D]); qsq = P.sc("qsq", [128, D])
        kvmod = [qt, qsq]
        hs = P.sc("hs", [128, 16]); rot = P.sc("rot", [128, 16, 16]); rtmp = P.sc("rtmp", [128, 16, 8])
        qTn = P.sc("qTn", [128, KC, 128], BF16)
        pexp = [P.sc("pexp%d" % i, [128, 8, 256], BF16) for i in range(2)]
        o_t = htmp; den = P.sc("den", [128, 16]); oT = P.sc("oT", [128, KC, 128], BF16)

        def rope_tables():
            P.dma("sp", posi.v, pos_in.v)
            P.copy(posf.v, posi.v)
            for f in range(8):
                P.memset(invf[:, :, f:f + 1], float(500000.0 ** (-(2 * f) / 16.0)), eng="pool")
            for shift, dst in ((0.0, sinb), (PI / 2, cosb)):
                P.tt(ang.v, invf.v, posf.v.rearrange("p (t o) -> p t o", o=1).bcast([128, NT, 8]), ALU.mult)
                if shift:
                    P.ts(ang.v, ang.v, shift, ALU.add)
                P.ts(angk.v, ang.v, 1.0 / TWO_PI, ALU.mult)
                P.copy(angi.v, angk.v)
                P.copy(angk.v, angi.v)
                P.stt(ang.v, angk.v, -TWO_PI, ang.v, ALU.mult, ALU.add)
                P.ts(angk.v, ang.v, PI, ALU.is_gt)
                P.stt(ang.v, angk.v, -TWO_PI, ang.v, ALU.mult, ALU.add)
                P.ts(angk.v, ang.v, -PI, ALU.is_lt)
                P.stt(ang.v, angk.v, TWO_PI, ang.v, ALU.mult, ALU.add)
                P.ts(ang.v, ang.v, PI, ALU.min, -PI, ALU.max)
                P.act(dst.v, ang.v, AF.Sin)

        def rms_rope(src, nh, gain, i, dst):
            s3 = src.rearrange("p (h d) -> p h d", d=64)
            d3 = dst.rearrange("p (h d) -> p h d", d=64)
            q3 = qsq[:, 0:nh * 64].rearrange("p (h d) -> p h d", d=64)
            P.tt(qsq[:, 0:nh * 64], src, src, ALU.mult, eng="pool")
            P.I("dve", lambda e: e.reduce_sum(out=hs[:, 0:nh].ap, in_=q3.ap, axis=mybir.AxisListType.X), [q3], [hs.v])
            rms_rstd(hs[:, 0:nh], hs[:, 0:nh], 64)
            P.tt(d3, s3, hs[:, 0:nh].rearrange("p (h o) -> p h o", o=1).bcast([128, nh, 64]), ALU.mult)
            P.tt(d3, d3, gain.v.rearrange("p (o d) -> p o d", o=1).bcast([128, nh, 64]), ALU.mult)
            cb = cosb[:, i, :].rearrange("p (o f) -> p o f", o=1).bcast([128, nh, 8])
            sb_ = sinb[:, i, :].rearrange("p (o f) -> p o f", o=1).bcast([128, nh, 8])
            x1, x2 = d3[:, :, 0:8], d3[:, :, 8:16]
            r = rot[:, 0:nh, :]
            P.tt(r[:, :, 0:8], x1, cb, ALU.mult)
            P.tt(rtmp[:, 0:nh, :], x2, sb_, ALU.mult)
            P.tt(r[:, :, 0:8], r[:, :, 0:8], rtmp[:, 0:nh, :], ALU.subtract)
            P.tt(r[:, :, 8:16], x2, cb, ALU.mult)
            P.tt(rtmp[:, 0:nh, :], x1, sb_, ALU.mult)
            P.tt(r[:, :, 8:16], r[:, :, 8:16], rtmp[:, 0:nh, :], ALU.add)
            P.copy(d3[:, :, 0:16], r, eng="pool")

        compute_mod(kv_ada_w, kv_ada_b, 0, [kvmod[0][:, 0:512], kvmod[0][:, 512:1024], kvmod[1][:, 0:512], kvmod[1][:, 512:1024]])
        gain_fold(kvmod[1], kv_norm_gain)
        P.dma("pool", kvw.v, wview(kv_w))
        P.dma("sp", kgain.v, pbc(k_norm_gain)); P.dma("sp", qgain.v, pbc(q_norm_gain)); P.dma("sp", sinkrep.v, pbc(b_sinks))
        P.act(sinkrep.v, sinkrep.v, AF.Exp)
        rope_tables()
        norm_all(kvmod[1], kvmod[0])
        for i in range(NT):
            isl = slice(i * 128, (i + 1) * 128)
            ps = next_pb()
            for kc in range(KC):
                P.mm(ps[:, 0:256], hT[:, kc, isl], kvw[:, kc, :], kc == 0, kc == KC - 1)
            P.copy(kvt.v, ps[:, 0:256], eng="act")
            P.memset(vext[i].v, 1.0, eng="pool")
            P.copy(vext[i][:, :, 0:64], kvt[:, 128:256].rearrange("p (g d) -> p g d", g=2))
            rms_rope(kvt[:, 0:128], 2, kgain, i, qt[:, 0:128])
            k3 = qt[:, 0:128].rearrange("p (g d) -> p g d", g=2)
            P.copy(kdup[:, :, 0:64], k3, eng="pool")
            P.copy(kdup[:, :, 64:128], k3, eng="pool")
            ps = next_pb()
            for g in range(2):
                P.tr(ps[:, g * 128:(g + 1) * 128], kdup[:, g, :], ident.v)
            P.copy(kTd[:, :, isl], ps[:, 0:256].rearrange("p (g t) -> p g t", g=2), eng="act")
        norm_all(A, B)
        wq2 = [piece[0], piece[1]]; wo2 = [piece[2], piece[3]]
        for hh in range(2):
            P.dma("pool", wq2[hh].v, wview(b_w_q)[:, :, hh * 512:(hh + 1) * 512])
            P.dma("pool", wo2[hh].v, wview(b_w_out)[:, :, hh * 512:(hh + 1) * 512])
            for kc in range(KC):
                P.tt(wo2[hh][:, kc, :], wo2[hh][:, kc, :], G[:, hh * 512:(hh + 1) * 512], ALU.mult, eng="pool")
        m3 = m01.v.rearrange("p (o k) -> p o k", o=1).bcast([128, 8, 256])
        for n in range(NT):
            nsl = slice(n * 128, (n + 1) * 128)
            psl = slice((n - 1) * 128, n * 128) if n > 0 else nsl
            for hh in range(2):
                ps = next_pb()
                for kc in range(KC):
                    P.mm(ps.v, hT[:, kc, nsl], wq2[hh][:, kc, :], kc == 0, kc == KC - 1)
                P.copy(qt[:, hh * 512:(hh + 1) * 512], ps.v, eng="act")
            rms_rope(qt.v, 16, qgain, n, hrow.v)
            transpose8(hrow.v, [(qTn.v, evac_eng())])
            for g in range(2):
                pe_ = pexp[g]
                for quad in range(2):
                    pss = [next_pb(), next_pb()]
                    for jj in range(4):
                        j = quad * 4 + jj
                        hq = g * 8 + j
                        kc, r0 = hq // 2, (hq % 2) * 64
                        ps = pss[jj % 2]
                        c0 = (jj // 2) * 256
                        P.mm(ps[:, c0:c0 + 128], kTd[r0:r0 + 64, g, psl], qTn[r0:r0 + 64, kc, :], True, True)
                        P.mm(ps[:, c0 + 128:c0 + 256], kTd[r0:r0 + 64, g, nsl], qTn[r0:r0 + 64, kc, :], True, True)
                    for jj in range(4):
                        c0 = (jj // 2) * 256
                        P.act(pe_[:, quad * 4 + jj, :], pss[jj % 2][:, c0:c0 + 256], AF.Exp, scale=0.125)
                P.tt(pe_.v, pe_.v, m3, ALU.mult)
                pos_ = [next_pb(), next_pb()]
                for j in range(8):
                    oc = pos_[j // 4][:, (j % 4) * 65:(j % 4 + 1) * 65]
                    if n > 0:
                        P.mm(oc, pe_[:, j, 0:128], vext[n - 1][:, g, :], True, False)
                        P.mm(oc, pe_[:, j, 128:256], vext[n][:, g, :], False, True)
                    else:
                        P.mm(oc, pe_[:, j, 128:256], vext[n][:, g, :], True, True)
                for q4 in range(2):
                    po3 = pos_[q4][:, 0:260].rearrange("p (j e) -> p j e", e=65)
                    dsl = slice(g * 8 + q4 * 4, g * 8 + q4 * 4 + 4)
                    P.tt(den[:, dsl].rearrange("p (j o) -> p j o", o=1), po3[:, :, 64:65],
                         sinkrep[:, dsl].rearrange("p (j o) -> p j o", o=1), ALU.add)
                    P.recip(den[:, dsl], den[:, dsl])
                    P.tt(o_t[:, (g * 8 + q4 * 4) * 64:(g * 8 + q4 * 4 + 4) * 64].rearrange("p (j d) -> p j d", d=64),
                         po3[:, :, 0:64], den[:, dsl].rearrange("p (j o) -> p j o", o=1).bcast([128, 4, 64]), ALU.mult)
            transpose8(o_t.v, [(oT.v, evac_eng())])
            for hh in range(2):
                ps = next_pb()
                for kc in range(KC):
                    P.mm(ps.v, oT[:, kc, :], wo2[hh][:, kc, :], kc == 0, kc == KC - 1)
                P.tt(xs[n][:, hh * 512:(hh + 1) * 512], xs[n][:, hh * 512:(hh + 1) * 512], ps.v, ALU.add)

    stages = [("mod0a", lambda: layer_mod(0, 0)), ("gdn", gdn), ("mod0b", lambda: layer_mod(0, 1)), ("moe0", lambda: moe(0)),
              ("mod1a", lambda: layer_mod(1, 0)), ("attn", layer1_mixer), ("mod1b", lambda: layer_mod(1, 1)),
              ("moe1", lambda: moe(1))]
    try:
        for name, fn in stages:
            fn()
            if stop_after == name:
                break
    except _Cut:
        pass
    P.scope()
    for i in range(NT):
        P.dma("sp", y_out[i * 128:(i + 1) * 128, :], xs[i].v, key=xs[i])
    P.emit()
    return nc, P


def make_in_maps(inputs, S=2048, NE=32):
    f = lambda a: np.ascontiguousarray(np.asarray(a, dtype=np.float32))
    B = inputs["x"].shape[0]
    NT = S // 128
    up_b = np.asarray(inputs["up_b"], dtype=np.float32)
    shared = {
        "ada_w": f(inputs["ada_w"]), "ada_b": f(inputs["ada_b"]), "norm_gain": f(inputs["norm_gain"]).reshape(4, D),
        "a_w_in": f(inputs["a_w_in"][0]), "a_convT": f(np.asarray(inputs["a_conv"][0]).T), "a_log": f(inputs["a_log"][0]),
        "a_dt_bias": f(inputs["a_dt_bias"][0]), "a_out_gain": f(inputs["a_out_gain"][0]), "a_w_out": f(inputs["a_w_out"][0]),
        "kv_ada_w": f(inputs["kv_ada_w"]), "kv_ada_b": f(inputs["kv_ada_b"]).reshape(1, -1),
        "kv_norm_gain": f(inputs["kv_norm_gain"]).reshape(1, -1),
        "kv_w": f(inputs["kv_w"]), "k_norm_gain": f(inputs["k_norm_gain"]), "b_w_q": f(inputs["b_w_q"][0]),
        "q_norm_gain": f(inputs["q_norm_gain"][0]), "b_sinks": f(inputs["b_sinks"][0]), "b_w_out": f(inputs["b_w_out"][0]),
        "router_w": f(inputs["router_w"]), "router_b": f(inputs["router_b"]), "up_w": f(inputs["up_w"]),
        "up_bT": f(up_b.reshape(2, up_b.shape[1], 16, 128).transpose(0, 1, 3, 2)),
        "down_w": f(inputs["down_w"]), "down_b": f(inputs["down_b"]),
    }
    maps = []
    for b in range(B):
        m = dict(shared)
        m["x"] = f(inputs["x"][b])
        m["cT"] = f(np.asarray(inputs["c"][b]).reshape(KC, 128).T)
        m["pos"] = np.ascontiguousarray(np.asarray(inputs["positions"][b], dtype=np.int32).reshape(NT, 128).T)
        maps.append(m)
    return maps


_CACHE = {}


def kernel(**inputs):
    if "nc" not in _CACHE:
        _CACHE["nc"] = build()[0]
    nc = _CACHE["nc"]
    maps = make_in_maps(inputs)
    res = run_bass_kernel_spmd(nc, maps, core_ids=list(range(len(maps))))
    return np.stack([np.asarray(r["y"], dtype=np.float32) for r in res.results], axis=0)
```

```python
import numpy as np
from contextlib import ExitStack
import concourse.bass as bass
import concourse.mybir as mybir
from concourse.bass_utils import run_bass_kernel_spmd

F32 = mybir.dt.float32
BF16 = mybir.dt.bfloat16
I32 = mybir.dt.int32
U32 = mybir.dt.uint32
AF = mybir.ActivationFunctionType
ALU = mybir.AluOpType
ENGINES = ("pe", "act", "dve", "pool", "sp")
NEG = -60000.0


class V:
    __slots__ = ("t", "ap")

    def __init__(self, t, ap):
        self.t, self.ap = t, ap

    def __getitem__(self, k):
        return V(self.t, self.ap[k])

    def bitcast(self, dt):
        return V(self.t, self.ap.bitcast(dt))

    def rearrange(self, s, **kw):
        return V(self.t, self.ap.rearrange(s, **kw))

    def bcast(self, shape):
        return V(self.t, self.ap.to_broadcast(list(shape)))


class T:
    _n = 0

    def __init__(self, ap, name):
        self.ap0 = ap
        self.name = name
        self.id = T._n
        T._n += 1
        self.last_writer = None
        self.readers = []
        self.psum = False

    def __getitem__(self, k):
        return V(self, self.ap0[k])

    @property
    def v(self):
        return V(self, self.ap0)


class Op:
    __slots__ = ("eng", "fn", "reads", "writes", "is_dma", "key", "deps", "signal",
                 "tok_sem", "tok_val", "idx", "sid")


class Prog:
    def __init__(self, nc):
        self.nc = nc
        self.ops = []
        self.stack = ExitStack()
        self.pending = {}
        self.last_eng_op = {}
        self.last_key_op = {}
        self.arena = None
        self.arena_off = 0
        self.arena_words = 0
        self.scope_id = 0

    def make_arena(self, words):
        self.arena = self.stack.enter_context(self.nc.sbuf_tensor("arena", [128, words], F32))
        self.arena_words = words

    def scope(self):
        deps = set(self.last_eng_op.values()) | set(self.last_key_op.values())
        self.pending = {e: set(deps) for e in ENGINES}
        self.arena_off = 0
        self.scope_id += 1

    def sc(self, name, shape, dtype=F32):
        n = 1
        for d in shape[1:]:
            n *= d
        words = (n + 1) // 2 if dtype == BF16 else n
        words = (words + 7) // 8 * 8
        assert self.arena_off + words <= self.arena_words, ("arena overflow", name, self.arena_off + words)
        ap = self.arena[0:shape[0], self.arena_off:self.arena_off + words]
        self.arena_off += words
        if dtype != F32:
            ap = ap.bitcast(dtype)
        ap = ap[:, 0:n]
        if len(shape) == 3:
            ap = ap.rearrange("p (a b) -> p a b", a=shape[1])
        return T(ap, name)

    def sb(self, name, shape, dtype=F32):
        t = self.stack.enter_context(self.nc.sbuf_tensor(name, list(shape), dtype))
        return T(t[tuple(slice(None) for _ in shape)], name)

    def ps(self, name, shape, dtype=F32):
        t = self.stack.enter_context(self.nc.psum_tensor(name, list(shape), dtype))
        tt = T(t[tuple(slice(None) for _ in shape)], name)
        tt.psum = True
        return tt

    def dram(self, name, shape, dtype=F32, kind="Internal"):
        t = self.nc.dram_tensor(name, list(shape), dtype, kind=kind)
        return T(t.ap(), name)

    def _add(self, eng, fn, reads, writes, is_dma=False, key=None):
        o = Op()
        o.eng, o.fn, o.is_dma, o.key = eng, fn, is_dma, key
        o.reads = list({t.id: t for t in reads}.values())
        o.writes = list({t.id: t for t in list(writes) + [t for t in reads if t.psum]}.values())
        o.deps, o.signal = set(), False
        i = len(self.ops)
        o.idx = i
        o.sid = self.scope_id
        for t in o.reads:
            if t.last_writer is not None:
                o.deps.add(t.last_writer)
        for t in o.writes:
            if t.last_writer is not None:
                o.deps.add(t.last_writer)
            o.deps.update(t.readers)
        for t in o.reads:
            t.readers.append(i)
        for t in o.writes:
            t.last_writer = i
            t.readers = []
        if self.pending.get(eng):
            o.deps |= self.pending.pop(eng)
        if is_dma:
            self.last_key_op[key.id] = i
        else:
            self.last_eng_op[eng] = i
        o.deps.discard(i)
        self.ops.append(o)
        return o

    def I(self, eng, fn, ins, outs):
        return self._add(eng, fn, [v.t for v in ins], [v.t for v in outs])

    def dma(self, eng, out, in_, key=None, **kw):
        k = key if key is not None else (out.t if out.t.name[0] != "@" else in_.t)
        return self._add(eng, lambda e: e.dma_start(out=out.ap, in_=in_.ap, **kw), [in_.t], [out.t],
                         is_dma=True, key=k)

    def mm(self, out, lhsT, rhs, start=True, stop=True, extra_reads=()):
        return self.I("pe", lambda e: e.matmul(out.ap, lhsT=lhsT.ap, rhs=rhs.ap, start=start, stop=stop),
                      [lhsT, rhs] + ([] if start else [out]) + list(extra_reads), [out])

    def tr(self, out, in_, ident):
        return self.I("pe", lambda e: e.transpose(out=out.ap, in_=in_.ap, identity=ident.ap), [in_, ident], [out])

    def act(self, out, in_, func, bias=None, scale=None, accum=None, eng="act"):
        ins = [in_] + ([bias] if isinstance(bias, V) else []) + ([scale] if isinstance(scale, V) else [])
        outs = [out] + ([accum] if accum is not None else [])

        def fn(e):
            kw = {}
            if bias is not None:
                kw["bias"] = bias.ap if isinstance(bias, V) else bias
            if scale is not None:
                kw["scale"] = scale.ap if isinstance(scale, V) else scale
            if accum is not None:
                kw["accum_out"] = accum.ap
            return e.activation(out=out.ap, in_=in_.ap, func=func, **kw)
        return self.I(eng, fn, ins, outs)

    def ts(self, out, in0, s1, op0, s2=None, op1=None, eng="dve"):
        ins = [in0] + [s for s in (s1, s2) if isinstance(s, V)]

        def fn(e):
            a1 = s1.ap if isinstance(s1, V) else s1
            a2 = s2.ap if isinstance(s2, V) else s2
            if op1 is None:
                return e.tensor_scalar(out=out.ap, in0=in0.ap, scalar1=a1, scalar2=None, op0=op0)
            return e.tensor_scalar(out=out.ap, in0=in0.ap, scalar1=a1, scalar2=a2, op0=op0, op1=op1)
        return self.I(eng, fn, ins, [out])

    def tt(self, out, in0, in1, op, eng="dve"):
        return self.I(eng, lambda e: e.tensor_tensor(out=out.ap, in0=in0.ap, in1=in1.ap, op=op), [in0, in1], [out])

    def stt(self, out, in0, scalar, in1, op0, op1):
        ins = [in0, in1] + ([scalar] if isinstance(scalar, V) else [])
        return self.I("dve", lambda e: e.scalar_tensor_tensor(
            out=out.ap, in0=in0.ap, scalar=(scalar.ap if isinstance(scalar, V) else scalar),
            in1=in1.ap, op0=op0, op1=op1), ins, [out])

    def copy(self, out, in_, eng="dve"):
        if eng == "act":
            return self.act(out, in_, AF.Copy)
        return self.I(eng, lambda e: e.tensor_copy(out=out.ap, in_=in_.ap), [in_], [out])

    def memset(self, out, val, eng="pool"):
        return self.I(eng, lambda e: e.memset(out.ap, val), [], [out])

    def recip(self, out, in_):
        return self.I("dve", lambda e: e.reciprocal(out=out.ap, in_=in_.ap), [in_], [out])

    def affsel(self, out, in_, pattern, cmp, fill, base, cm):
        return self.I("pool", lambda e: e.affine_select(out=out.ap, in_=in_.ap, pattern=pattern, compare_op=cmp,
                                                        fill=fill, base=base, channel_multiplier=cm), [in_], [out])

    def emit(self):
        nc, ops = self.nc, self.ops
        for o in ops:
            keep = set()
            for d in o.deps:
                p = ops[d]
                if not p.is_dma and not o.is_dma and p.eng == o.eng:
                    if o.eng == "pe":
                        continue
                keep.add(d)
            o.deps = keep
            for d in keep:
                ops[d].signal = True
        eng_cnt = {e: 0 for e in ENGINES}
        key_cnt, sems = {}, {}
        slot_of, nslot = {}, {}
        for o in ops:
            if o.is_dma:
                sw = o.eng == "pool"
                sk = (o.sid, o.key.id, sw)
                if sk not in slot_of:
                    n = nslot.get((o.sid, sw), 0)
                    slot_of[sk] = (sw, n)
                    nslot[(o.sid, sw)] = n + 1
                k = slot_of[sk]
                key_cnt[k] = key_cnt.get(k, 0) + 16
                o.tok_sem, o.tok_val = ("k", k), key_cnt[k]
            elif o.signal:
                eng_cnt[o.eng] += 1
                o.tok_sem, o.tok_val = ("e", o.eng), eng_cnt[o.eng]
        for e in ENGINES:
            sems[("e", e)] = self.stack.enter_context(nc.semaphore("sem_" + e))
        for k in key_cnt:
            sems[("k", k)] = self.stack.enter_context(nc.semaphore("semk_%d_%d" % (int(k[0]), k[1])))
        self.n_sems = len(sems)
        final = [(("k", k), v) for k, v in key_cnt.items()] + [(("e", e), v) for e, v in eng_cnt.items() if v > 0]
        with nc.Block() as block:
            def mk(en):
                def body(eng):
                    waited = {}
                    for o in ops:
                        if o.eng != en:
                            continue
                        need = {}
                        for d in o.deps:
                            p = ops[d]
                            if p.tok_val > need.get(p.tok_sem, 0):
                                need[p.tok_sem] = p.tok_val
                        for s, v in need.items():
                            if waited.get(s, 0) >= v:
                                continue
                            eng.wait_ge(sems[s], v)
                            waited[s] = v
                        ins = o.fn(eng)
                        if o.is_dma:
                            ins.then_inc(sems[o.tok_sem], 16)
                        elif o.signal:
                            ins.then_inc(sems[o.tok_sem], 1)
                    if en == "sp":
                        for s, v in final:
                            if waited.get(s, 0) < v:
                                eng.wait_ge(sems[s], v)
                return body
            block.tensor(mk("pe"))
            block.scalar(mk("act"))
            block.vector(mk("dve"))
            block.gpsimd(mk("pool"))
            block.sync(mk("sp"))
        self.stack.close()


D = 1024
KC = 8
EPS = 1e-6
ARENA_WORDS = 19840


class _Cut(Exception):
    pass


def build(S=2048, NE=32, stop_after=None, cut=None):
    NT = S // 128
    TQ = min(S, 512)
    NQ = S // TQ
    CPQ = TQ // 128
    nc = bass.Bass("TRN2", target_bir_lowering=False)
    P = Prog(nc)

    def din(name, shape, dtype=F32):
        t = P.dram(name, shape, dtype, kind="ExternalInput")
        t.name = "@" + name
        return t

    x_in = din("x", [S, D]); cT_in = din("cT", [128, KC]); pos_in = din("pos", [128, NT], I32)
    ada_w = din("ada_w", [2, D, 6 * D]); ada_b = din("ada_b", [2, 6 * D]); norm_gain = din("norm_gain", [4, D])
    a_w_in = din("a_w_in", [D, 4112]); a_convT = din("a_convT", [3072, 4]); a_log = din("a_log", [8])
    a_dt_bias = din("a_dt_bias", [8]); a_out_gain = din("a_out_gain", [128]); a_w_out = din("a_w_out", [D, D])
    kv_ada_w = din("kv_ada_w", [D, 2 * D]); kv_ada_b = din("kv_ada_b", [1, 2 * D]); kv_norm_gain = din("kv_norm_gain", [1, D])
    kv_w = din("kv_w", [D, 256]); k_norm_gain = din("k_norm_gain", [64]); b_w_q = din("b_w_q", [D, D])
    q_norm_gain = din("q_norm_gain", [64]); b_sinks = din("b_sinks", [16]); b_w_out = din("b_w_out", [D, D])
    router_w = din("router_w", [2, D, NE]); router_b = din("router_b", [2, NE])
    up_w = din("up_w", [2, NE, D, 2 * D]); up_bT = din("up_bT", [2, NE, 128, 16])
    down_w = din("down_w", [2, NE, D, D]); down_b = din("down_b", [2, NE, D])
    y_out = P.dram("y", [S, D], F32, kind="ExternalOutput"); y_out.name = "@y"

    def ck(n):
        if cut is not None and cut == n:
            raise _Cut()

    def wview(v):
        v = v.v if isinstance(v, T) else v
        return v.rearrange("(kc p) f -> p kc f", p=128)

    def pbc(t, idx=None):
        ap = t.ap0 if idx is None else t.ap0[idx]
        return V(t, ap.partition_broadcast(128))

    xs = [P.sb("x%d" % i, [128, D]) for i in range(NT)]
    hT = P.sb("hT", [128, KC, S], BF16)
    ident = P.sb("ident", [128, 128]); ones = P.sb("ones", [128, 128])
    Uincl = P.sb("Uincl", [128, 128])
    MA = P.sb("MA", [128, 128]); zeros = P.sb("zeros", [128, 128])
    m01 = P.sb("m01", [128, 256], BF16)
    modt = [P.sb("mod%d" % j, [128, D]) for j in range(3)]
    crep = P.sb("crep", [128, KC, 128], BF16)
    cact = P.sb("cact", [128, KC])
    rowb = P.sb("rowb", [1, 512]); rowg = P.sb("rowg", [1, 512])
    NPC = 4
    piece = []
    pc_i = [0]

    def alloc_pieces():
        piece[:] = [P.sc("piece%d" % i, [128, KC, 512], BF16) for i in range(NPC)]
        pc_i[0] = 0
    hrow = P.sb("hrow", [128, D]); htmp = P.sb("htmp", [128, D]); ssq = P.sb("ssq", [128, 4])
    gates = [P.sb("gates%d" % i, [128, NE]) for i in range(NT)]
    gks = [P.sb("gks%d" % i, [128, 4]) for i in range(NT)]
    idxs = [P.sb("idxs%d" % i, [128, 4], I32) for i in range(NT)]
    zeros_row = P.sb("zeros_row", [1, 512])
    xg = P.dram("xg", [NE * (S // 2) + 1, D], BF16); xg.name = "@xg"
    yg = P.dram("yg", [NE * (S // 2) + 1, D], F32); yg.name = "@yg"
    P.make_arena(ARENA_WORDS)

    def next_piece():
        p = piece[pc_i[0] % NPC]
        pc_i[0] += 1
        return p
    pb = [P.ps("pb%d" % i, [128, 512]) for i in range(8)]
    pb_i = [0]

    def next_pb():
        p = pb[pb_i[0] % 8]
        pb_i[0] += 1
        return p
    ev_i = [0]

    def evac_eng():
        ev_i[0] += 1
        return "act" if ev_i[0] % 2 else "dve"

    P.memset(ones.v, 1.0); P.memset(zeros.v, 0.0); P.memset(zeros_row.v, 0.0)
    P.affsel(ident.v, ones.v, [[1, 128]], ALU.is_equal, 0.0, 0, -1)
    P.affsel(Uincl.v, ones.v, [[1, 128]], ALU.is_ge, 0.0, 0, -1)
    P.affsel(MA.v, zeros.v, [[-1, 128]], ALU.is_gt, NEG, 0, 1)
    P.affsel(hrow[:, 0:128], ones.v, [[-1, 128]], ALU.is_gt, 0.0, 0, 1)
    P.copy(m01[:, 0:128], hrow[:, 0:128], eng="pool")
    P.copy(m01[:, 128:256], Uincl.v, eng="pool")

    for i in range(NT):
        P.dma("sp", xs[i].v, x_in[i * 128:(i + 1) * 128, :])
    P.dma("sp", cact.v, cT_in.v)
    P.act(cact.v, cact.v, AF.Silu)
    for kc in range(KC):
        P.ts(crep[:, kc, :], ones.v, cact[:, kc:kc + 1], ALU.mult)

    def compute_mod(wdram, brow, col0, outs):
        for n, o in enumerate(outs):
            c0 = col0 + n * 512
            pc = next_piece()
            P.dma("pool", pc.v, wview(wdram)[:, :, c0:c0 + 512])
            P.dma("sp", rowb.v, brow[0:1, c0:c0 + 512])
            ps = next_pb()
            P.mm(ps.v, ones[0:1, :], rowb.v, True, False)
            for kc in range(KC):
                P.mm(ps.v, crep[:, kc, :], pc[:, kc, :], False, kc == KC - 1)
            P.copy(o, ps.v, eng=evac_eng())

    def gain_fold(dst, grow):
        for hh in range(2):
            P.dma("sp", rowg.v, grow[0:1, hh * 512:(hh + 1) * 512])
            ps = next_pb()
            P.mm(ps.v, ones[0:1, :], rowg.v, True, True)
            P.stt(dst[:, hh * 512:(hh + 1) * 512], dst[:, hh * 512:(hh + 1) * 512], 1.0, ps.v, ALU.add, ALU.mult)

    def layer_mod(l, half):
        P.scope()
        alloc_pieces()
        outs = []
        for j in range(3):
            outs += [modt[j][:, 0:512], modt[j][:, 512:1024]]
        compute_mod(ada_w[l], ada_b[l:l + 1, :], half * 3 * D, outs)
        gain_fold(modt[1], norm_gain[2 * l + half:2 * l + half + 1, :])

    def rms_rstd(dst, src_sum, n):
        P.ts(dst, src_sum, 1.0 / n, ALU.mult, EPS, ALU.add)
        P.act(dst, dst, AF.Sqrt)
        P.recip(dst, dst)

    def norm_mod_tile(i, A, B, out_f32):
        P.act(htmp.v, xs[i].v, AF.Square, accum=ssq[:, 0:1])
        rms_rstd(ssq[:, 1:2], ssq[:, 0:1], D)
        P.stt(htmp.v, xs[i].v, ssq[:, 1:2], A.v, ALU.mult, ALU.mult)
        P.tt(out_f32, htmp.v, B.v, ALU.add, eng="pool")

    def transpose8(src_f32, dsts):
        for half in range(2):
            ps = next_pb()
            for q in range(4):
                kc = half * 4 + q
                P.tr(ps[:, q * 128:(q + 1) * 128], src_f32[:, kc * 128:(kc + 1) * 128], ident.v)
            for dv, eng in dsts:
                P.copy(dv[:, half * 4:half * 4 + 4, :], ps.v.rearrange("p (q t) -> p q t", q=4), eng=eng)

    def norm_all(A, B):
        for i in range(NT):
            norm_mod_tile(i, A, B, hrow.v)
            transpose8(hrow.v, [(hT[:, :, i * 128:(i + 1) * 128], evac_eng())])

    bc_cache = {}

    def bc_reg(e, val):
        if val not in bc_cache:
            bc_cache[val] = e.to_reg(val)
        return bc_cache[val]

    def moe(l):
        B, A, G = modt
        CAP = S // 2
        NB = CAP // 128
        NH = max(CAP // 512, 1)
        HW_ = min(CAP, 512)
        TRASH = NE * CAP
        P.scope()
        h2T_f = P.sc("h2Tf", [128, KC, 128]); rw_sb = P.sc("rw_sb", [128, KC, NE]); rbrep = P.sc("rbrep", [128, NE])
        lg = P.sc("lg", [128, NE]); lgr = P.sc("lgr", [128, NE]); top8 = P.sc("top8", [128, 8]); msk = P.sc("msk", [128, NE])
        sm = P.sc("sm", [128, 4]); idx8 = P.sc("idx8", [128, 8], U32); ef = P.sc("ef", [128, 4])
        db_sb = P.sc("db_sb", [NE, D]); gT = P.sc("gT", [NE, 128]); gpad = P.sc("gpad", [128, 128])
        runsum = P.sc("runsum", [128, NE]); pos = P.sc("pos", [128, NE]); ov = P.sc("ov", [128, NE]); t1 = P.sc("t1", [128, NE])
        iota_e = P.sc("iota_e", [128, NE]); base_e = P.sc("base_e", [128, NE]); iota_i = P.sc("iota_i", [128, NE], I32)
        oh = P.sc("oh", [128, NE]); ohj = P.sc("ohj", [128, NE]); destf = P.sc("destf", [128, 4])
        Ustr = P.sc("Ustr", [128, 128])
        h2b = [P.sc("h2b%d" % j, [128, D], BF16) for j in range(2)]
        zt = P.sc("zt", [128, 4096], BF16)
        P.memset(gpad.v, 0.0)
        P.memset(runsum.v, 0.0)
        P.tt(Ustr.v, Uincl.v, ident.v, ALU.subtract, eng="pool")
        for e_ in range(NE):
            P.memset(base_e[:, e_:e_ + 1], float(e_ * CAP), eng="pool")
        if l == 0:
            P.memset(zt.v, 0.0, eng="pool")
            rows_per = 128 * 4
            for r0 in range(0, NE * CAP, rows_per):
                P.dma("sp", xg[r0:r0 + rows_per, :].rearrange("(p a) d -> p (a d)", p=128), zt.v, key=zt)
            P.dma("sp", xg[TRASH:TRASH + 1, :], zt[0:1, 0:D], key=zt)
            P.copy(lg[0:1, 0:NE], zt[0:1, 0:NE])
            for q in range(2):
                P.dma("sp", yg[TRASH:TRASH + 1, q * 512:(q + 1) * 512], zeros_row.v, key=zeros_row)
        P.dma("sp", rw_sb.v, wview(router_w[l]))
        P.dma("sp", rbrep.v, pbc(router_b, l))
        P.dma("sp", db_sb.v, down_b[l])
        P.tt(db_sb.v, db_sb.v, G[0:NE, :], ALU.mult, eng="pool")
        for i in range(NT):
            norm_mod_tile(i, A, B, hrow.v)
            hb = h2b[i % 2]
            P.copy(hb.v, hrow.v, eng="pool")
            transpose8(hrow.v, [(h2T_f.v, evac_eng())])
            ps = next_pb()
            for kc in range(KC):
                P.mm(ps[:, 0:NE], h2T_f[:, kc, :], rw_sb[:, kc, :], kc == 0, kc == KC - 1)
            P.tt(lgr.v, ps[:, 0:NE], rbrep.v, ALU.add)
            P.I("dve", lambda e: e.max(out=top8.v.ap, in_=lgr.v.ap), [lgr.v], [top8.v])
            P.ts(msk.v, lgr.v, top8[:, 3:4], ALU.is_ge)
            P.ts(sm[:, 0:1], top8[:, 0:1], -1.0, ALU.mult)
            P.act(lg.v, lgr.v, AF.Exp, bias=sm[:, 0:1], scale=1.0)
            P.tt(lg.v, lg.v, msk.v, ALU.mult)
            P.I("dve", lambda e: e.reduce_sum(out=sm[:, 1:2].ap, in_=lg.v.ap, axis=mybir.AxisListType.X),
                [lg.v], [sm[:, 1:2]])
            P.recip(sm[:, 2:3], sm[:, 1:2])
            P.ts(gpad[:, 0:NE], lg.v, sm[:, 2:3], ALU.mult)
            P.copy(gates[i].v, gpad[:, 0:NE], eng="pool")
            P.act(gks[i].v, top8[:, 0:4], AF.Exp, bias=sm[:, 0:1], scale=1.0)
            P.ts(gks[i].v, gks[i].v, sm[:, 2:3], ALU.mult)
            ps = next_pb()
            P.tr(ps[:, 0:128], gpad.v, ident.v)
            P.copy(gT.v, ps[0:NE, 0:128], eng="act")
            for hh in range(2):
                ps = next_pb()
                P.mm(ps.v, gT.v, db_sb[:, hh * 512:(hh + 1) * 512], True, True)
                P.tt(xs[i][:, hh * 512:(hh + 1) * 512], xs[i][:, hh * 512:(hh + 1) * 512], ps.v, ALU.add)
            ps = next_pb()
            P.mm(ps[:, 0:NE], Ustr.v, msk.v, True, False)
            P.mm(ps[:, 0:NE], ones.v, runsum.v, False, True)
            P.copy(pos.v, ps[:, 0:NE], eng="act")
            P.tt(runsum.v, runsum.v, msk.v, ALU.add, eng="pool")
            P.ts(ov.v, pos.v, float(CAP), ALU.is_ge)
            P.tt(pos.v, pos.v, base_e.v, ALU.add)
            P.tt(t1.v, pos.v, ov.v, ALU.mult)
            P.tt(pos.v, pos.v, t1.v, ALU.subtract)
            P.stt(pos.v, ov.v, float(TRASH), pos.v, ALU.mult, ALU.add)
            for k in range(4):
                P.ts(oh.v, lgr.v, top8[:, k:k + 1], ALU.is_equal)
                P.tt(ohj.v, oh.v, pos.v, ALU.mult)
                P.I("dve", lambda e, k=k: e.reduce_sum(out=destf[:, k:k + 1].ap, in_=ohj.v.ap, axis=mybir.AxisListType.X),
                    [ohj.v], [destf.v])
            P.copy(idxs[i].v, destf.v)
            for k in range(4):
                P._add("pool", (lambda e, k=k, hb=hb, ix=idxs[i]: e.indirect_dma_start(
                    out=xg.ap0[:, :], out_offset=bass.IndirectOffsetOnAxis(ap=ix[:, k:k + 1].ap, axis=0),
                    in_=hb.v.ap, in_offset=None, bounds_check=bc_reg(e, TRASH), oob_is_err=False)),
                    [hb, idxs[i]], [xg], is_dma=True, key=hb)
        P.scope()
        alloc_pieces()
        xT = T(hT.ap0[:, :, 0:CAP], "xT"); actT = T(hT.ap0[:, :, CAP:2 * CAP], "actT")
        xin = [P.sc("xin%d" % j, [128, D], BF16) for j in range(NB)]

        def load_x(e_):
            for b_ in range(NB):
                P.dma("sp", xin[b_].v, xg[e_ * CAP + b_ * 128: e_ * CAP + (b_ + 1) * 128, :])
        ub = P.sc("ub", [128, 16]); ub1 = P.sc("ub1", [128, 8])
        gact = [P.sc("gact%d" % j, [128, HW_]) for j in range(2)]
        sgm = [P.sc("sgm%d" % j, [128, HW_], BF16) for j in range(2)]
        gsb = [P.sc("gsb%d" % j, [128, HW_], BF16) for j in range(2)]
        lin1 = [P.sc("lin1%d" % j, [128, HW_]) for j in range(2)]
        ybuf = [P.sc("ybuf%d" % j, [128, D]) for j in range(2)]
        identb = P.sc("identb", [128, 128], BF16)
        P.copy(identb.v, ident.v, eng="pool")
        cnt = 0
        load_x(0)
        for e in range(NE):
            P.dma("sp", ub.v, up_bT[l, e])
            P.ts(ub1.v, ub[:, 8:16], 1.0, ALU.add)
            for b in range(NB):
                xi = xin[b]
                ps = next_pb()
                psb = ps.v.bitcast(BF16)
                for kc in range(KC):
                    P.tr(psb[:, kc * 128:(kc + 1) * 128], xi[:, kc * 128:(kc + 1) * 128], identb.v)
                P.copy(xT[:, :, b * 128:(b + 1) * 128], psb.rearrange("p (k t) -> p k t", k=KC), eng="act")
            if e + 1 < NE:
                load_x(e + 1)
            pds = []
            for quad in range(2):
                pg, pl = next_piece(), next_piece()
                P.dma("pool", pg.v, wview(up_w[l, e])[:, :, quad * 512:(quad + 1) * 512])
                P.dma("pool", pl.v, wview(up_w[l, e])[:, :, 1024 + quad * 512:1024 + (quad + 1) * 512])
                for mm_ in range(4):
                    m = quad * 4 + mm_
                    for hf in range(NH):
                        ssl = slice(hf * HW_, (hf + 1) * HW_)
                        bb = cnt % 2
                        cnt += 1
                        psg, psl = next_pb(), next_pb()
                        for kc in range(KC):
                            P.mm(psg[:, 0:HW_], pg[:, kc, mm_ * 128:(mm_ + 1) * 128], xT[:, kc, ssl], kc == 0, kc == KC - 1)
                        for kc in range(KC):
                            P.mm(psl[:, 0:HW_], pl[:, kc, mm_ * 128:(mm_ + 1) * 128], xT[:, kc, ssl], kc == 0, kc == KC - 1)
                        P.ts(gact[bb].v, psg[:, 0:HW_], ub[:, m:m + 1], ALU.add, 7.0, ALU.min)
                        P.act(sgm[bb].v, gact[bb].v, AF.Sigmoid, scale=1.702)
                        P.tt(gsb[bb].v, gact[bb].v, sgm[bb].v, ALU.mult)
                        P.ts(lin1[bb].v, psl[:, 0:HW_], ub1[:, m:m + 1], ALU.add, 8.0, ALU.min)
                        P.stt(actT[:, m, ssl], lin1[bb].v, -6.0, gsb[bb].v, ALU.max, ALU.mult)
            for quad in range(2):
                pd = next_piece()
                dsrc = down_w[l, e][quad * 512:(quad + 1) * 512, :].rearrange("(c p) f -> p c f", p=128)
                pdv = pd.v.rearrange("p a b -> p (a b)").rearrange("p (c f) -> p c f", c=4)
                P.dma("pool", pdv, dsrc)
                pds.append(pdv)
            for b in range(NB):
                yb = ybuf[b % 2]
                for hh in range(2):
                    ps = next_pb()
                    for m in range(8):
                        P.mm(ps.v, actT[:, m, b * 128:(b + 1) * 128], pds[m // 4][:, m % 4, hh * 512:(hh + 1) * 512],
                             m == 0, m == 7)
                    P.copy(yb[:, hh * 512:(hh + 1) * 512], ps.v, eng="act")
                P.dma("sp", yg[e * CAP + b * 128: e * CAP + (b + 1) * 128, :], yb.v, key=yb)
        P.scope()
        yks = [[P.sc("yk%d_%d" % (r, j), [128, D]) for j in range(4)] for r in range(3)]
        accs = [P.sc("acc%d" % r, [128, D]) for r in range(2)]
        for i in range(NT):
            yk = yks[i % 3]
            for k in range(4):
                P._add("pool", (lambda e, k=k, ix=idxs[i], yk=yk: e.indirect_dma_start(
                    out=yk[k].v.ap, out_offset=None, in_=yg.ap0[:, :],
                    in_offset=bass.IndirectOffsetOnAxis(ap=ix[:, k:k + 1].ap, axis=0), bounds_check=bc_reg(e, TRASH), oob_is_err=False)),
                    [yg, idxs[i]], [yk[k]], is_dma=True, key=yk[k])
            acc = accs[i % 2]
            P.ts(acc.v, yk[0].v, gks[i][:, 0:1], ALU.mult)
            for k in range(1, 4):
                P.stt(acc.v, yk[k].v, gks[i][:, k:k + 1], acc.v, ALU.mult, ALU.add)
            P.tt(acc.v, acc.v, G.v, ALU.mult, eng="pool")
            P.tt(xs[i].v, xs[i].v, acc.v, ALU.add)

    H = 8
    NCH = NT
    LNSC = float(np.log(128.0 ** -0.5))

    def gdn():
        B, A, G = modt
        norm_all(A, B)
        ck(1)
        P.scope()
        sq = lambda n: P.sc(n, [128, 128])
        ab_sb = P.sc("ab_sb", [128, KC, 16], BF16)
        sc_a = P.sc("sc_a", [128, NCH, 8]); sc_beta = P.sc("sc_beta", [128, NCH, 8]); sc_nbeta = P.sc("sc_nbeta", [128, NCH, 8])
        sc_g = P.sc("sc_g", [128, NCH, 8]); sc_gc = P.sc("sc_gc", [128, NCH, 8]); sc_egc = P.sc("sc_egc", [128, NCH, 8])
        sc_etail = P.sc("sc_etail", [128, NCH, 8]); sc_gtot = P.sc("sc_gtot", [128, NCH, 8]); sc_ng = P.sc("sc_ng", [128, NCH, 8])
        dtb = P.sc("dtb", [128, 8]); alog = P.sc("alog", [128, 8]); gainrep = sq("gainrep")
        TQG = 128; NQG = S // TQG; CPQG = TQG // 128

        def mkbufs(j):
            n = lambda s_: "%s_%d" % (s_, j)
            sqj = lambda s_: P.sc(n(s_), [128, 128])
            return ([P.sc(n("wqh%d" % q), [128, KC, 128], BF16) for q in range(4)], P.sc(n("wout_h"), [128, D], BF16),
                    P.sc(n("convw"), [128, 3, 4]), [P.sc(n("pre%d" % q), [128, TQG + 3]) for q in range(3)],
                    P.sc(n("cvt"), [128, TQG]), [P.sc(n("qkv%d" % q), [128, TQG]) for q in range(3)],
                    sqj("sqb"), P.sc(n("rn"), [128, 8]), P.sc(n("fac"), [128, 8]),
                    sqj("gU"), sqj("gUq"), sqj("gUk"), sqj("Elow"), sqj("Eincl"), sqj("egq"),
                    sqj("Nm0"), sqj("NmT0"), sqj("PTm0"),
                    sqj("QKm"), sqj("QKmT"), sqj("qdT"), sqj("vb"), sqj("kbg"), sqj("ktail"),
                    sqj("Sst"), sqj("o_sb"), sqj("z_sb"), sqj("og"),
                    P.sc(n("ogT"), [128, 128], BF16))
        KH = 3
        bufs = [mkbufs(j) for j in range(KH)]
        P.dma("pool", ab_sb.v, wview(a_w_in)[:, :, 4096:4112])
        P.dma("sp", dtb.v, pbc(a_dt_bias)); P.dma("sp", alog.v, pbc(a_log)); P.dma("sp", gainrep.v, pbc(a_out_gain))
        P.act(alog.v, alog.v, AF.Exp)
        ck(2)
        for c in range(NCH):
            ps = next_pb()
            for kc in range(KC):
                P.mm(ps[:, 0:16], hT[:, kc, c * 128:(c + 1) * 128], ab_sb[:, kc, :], kc == 0, kc == KC - 1)
            P.tt(sc_a[:, c, :], ps[:, 0:8], dtb.v, ALU.add)
            P.act(sc_beta[:, c, :], ps[:, 8:16], AF.Sigmoid)
        allc = lambda t: t.v.rearrange("p c h -> p (c h)")
        P.act(allc(sc_a), allc(sc_a), AF.Exp)
        P.act(allc(sc_a), allc(sc_a), AF.Ln, bias=1.0, scale=1.0)
        for c in range(NCH):
            P.stt(sc_g[:, c, :], sc_a[:, c, :], -1.0, alog.v, ALU.mult, ALU.mult)
        P.ts(allc(sc_ng), allc(sc_g), -1.0, ALU.mult)
        P.ts(allc(sc_nbeta), allc(sc_beta), -1.0, ALU.mult)
        for c in range(NCH):
            ps = next_pb()
            P.mm(ps[:, 0:8], Uincl.v, sc_g[:, c, :], True, True)
            P.mm(ps[:, 8:16], ones.v, sc_g[:, c, :], True, True)
            P.copy(sc_gc[:, c, :], ps[:, 0:8], eng="dve")
            P.act(sc_egc[:, c, :], ps[:, 0:8], AF.Exp)
            P.act(sc_gtot[:, c, :], ps[:, 8:16], AF.Exp)
            P.tt(sc_etail[:, c, :], ps[:, 8:16], sc_gc[:, c, :], ALU.subtract)
        P.act(allc(sc_etail), allc(sc_etail), AF.Exp)
        ck(3)
        def head_gen(h, b):
            (wq_h, wout_h, convw, pre, cvt, qkv, sqb, rn, fac, gU, gUq, gUk, Elow, Eincl, egq, Nm0, NmT0, PTm0,
             QKm, QKmT, qdT, vb, kbg, ktail, Sst, o_sb, z_sb, og, ogT) = b
            Nm = [Nm0, gU]; NmT = [NmT0, gUq]; PTm = [PTm0, gUk]
            u_sb, wT, vnew = Elow, Eincl, egq
            wout_f = wout_h
            for j in range(4):
                P.dma("pool", wq_h[j].v, wview(a_w_in)[:, :, j * 1024 + h * 128: j * 1024 + (h + 1) * 128])
            P.dma("sp", convw.v, V(a_convT, a_convT.ap0.rearrange("(j h p) k -> h p j k", j=3, p=128)[h]))
            P.dma("pool", wout_h.v, a_w_out[h * 128:(h + 1) * 128, :])
            P.tt(wout_f.v, wout_h.v, G.v, ALU.mult, eng="pool")
            P.memset(Sst.v, 0.0, eng="pool")
            ck(4)
            for tq in range(NQG):
                for j in range(3):
                    if tq == 0:
                        P.memset(pre[j][:, 0:3], 0.0, eng="pool")
                    else:
                        P.copy(pre[j][:, 0:3], pre[j][:, TQG:TQG + 3], eng="pool")
                    ps = next_pb()
                    for kc in range(KC):
                        P.mm(ps[:, 0:TQG], wq_h[j][:, kc, :], hT[:, kc, tq * TQG:(tq + 1) * TQG], kc == 0, kc == KC - 1)
                    P.copy(pre[j][:, 3:3 + TQG], ps[:, 0:TQG], eng="act")
                    P.ts(cvt.v, pre[j][:, 0:TQG], convw[:, j, 0:1], ALU.mult)
                    for k in range(1, 4):
                        P.stt(cvt.v, pre[j][:, k:k + TQG], convw[:, j, k:k + 1], cvt.v, ALU.mult, ALU.add)
                    P.act(qkv[j].v, cvt.v, AF.Silu)
                    yield
                ck(5)
                for cc in range(CPQG):
                    c = tq * CPQG + cc
                    lsl = slice(cc * 128, (cc + 1) * 128)
                    csl = slice(c * 128, (c + 1) * 128)
                    qc, kcn, vc = qkv[0][:, lsl], qkv[1][:, lsl], qkv[2][:, lsl]
                    bcol = lambda t: t[:, c, h:h + 1]
                    ps = next_pb()
                    P.act(sqb.v, qc, AF.Square)
                    P.mm(ps[:, 0:1], sqb.v, ones[:, 0:1], True, True)
                    P.act(sqb.v, kcn, AF.Square)
                    P.mm(ps[:, 1:2], sqb.v, ones[:, 0:1], True, True)
                    P.ts(rn[:, 0:2], ps[:, 0:2], EPS, ALU.add)
                    yield
                    P.act(rn[:, 4:6], rn[:, 0:2], AF.Ln)
                    P.ts(rn[:, 4:5], rn[:, 4:5], -0.5, ALU.mult, LNSC, ALU.add)
                    P.ts(rn[:, 5:6], rn[:, 5:6], -0.5, ALU.mult)
                    P.act(rn[:, 2:4], rn[:, 4:6], AF.Exp)
                    ck(6)
                    P.tt(fac[:, 0:1], bcol(sc_nbeta), rn[:, 3:4], ALU.mult)
                    P.tt(fac[:, 1:2], bcol(sc_beta), bcol(sc_egc), ALU.mult)
                    P.tt(fac[:, 1:2], fac[:, 1:2], rn[:, 3:4], ALU.mult)
                    P.tt(fac[:, 2:3], bcol(sc_etail), rn[:, 3:4], ALU.mult)
                    yield
                    P.ts(gU.v, Uincl.v, bcol(sc_ng), ALU.mult)
                    P.stt(gUk.v, ident.v, rn[:, 5:6], gU.v, ALU.mult, ALU.add)
                    P.ts(gUq.v, Uincl.v, bcol(sc_g), ALU.mult)
                    P.stt(gUq.v, ident.v, rn[:, 4:5], gUq.v, ALU.mult, ALU.add)
                    yield
                    p1 = next_pb()
                    P.mm(p1[:, 0:128], ones.v, gUk.v, True, False)
                    P.mm(p1[:, 0:128], ident.v, MA.v, False, True)
                    P.mm(p1[:, 128:256], ones.v, gUq.v, True, True)
                    yield
                    P.act(Elow.v, p1[:, 0:128], AF.Exp, bias=bcol(sc_gc), scale=1.0)
                    P.act(egq.v, p1[:, 128:256], AF.Exp)
                    P.tt(qdT.v, qc, egq.v, ALU.mult)
                    P.stt(Eincl.v, ident.v, rn[:, 3:4], Elow.v, ALU.mult, ALU.add)
                    yield
                    ck(7)
                    p2 = next_pb()
                    P.mm(p2[:, 0:128], kcn, kcn, True, True)
                    P.mm(p2[:, 128:256], qc, kcn, True, True)
                    yield
                    P.stt(Nm[0].v, p2[:, 0:128], fac[:, 0:1], Elow.v, ALU.mult, ALU.mult)
                    P.stt(QKm.v, p2[:, 128:256], rn[:, 2:3], Eincl.v, ALU.mult, ALU.mult)
                    yield
                    ck(71)
                    p3 = next_pb()
                    P.tr(p3[:, 0:128], Nm[0].v, ident.v)
                    P.tr(p3[:, 128:256], QKm.v, ident.v)
                    P.tr(p3[:, 256:384], kcn, ident.v)
                    P.tr(p3[:, 384:512], vc, ident.v)
                    yield
                    ck(72)
                    P.copy(NmT[0].v, p3[:, 0:128], eng="act")
                    P.copy(QKmT.v, p3[:, 128:256], eng="act")
                    P.tt(PTm[0].v, p3[:, 0:128], ident.v, ALU.add)
                    ck(73)
                    P.ts(kbg.v, p3[:, 256:384], fac[:, 1:2], ALU.mult)
                    P.ts(ktail.v, p3[:, 256:384], fac[:, 2:3], ALU.mult)
                    P.ts(vb.v, p3[:, 384:512], bcol(sc_beta), ALU.mult)
                    yield
                    ck(8)
                    cur = 0
                    for lev in range(6):
                        nxt = 1 - cur
                        p4 = next_pb()
                        P.mm(p4[:, 0:128], NmT[cur].v, Nm[cur].v, True, True)
                        if lev < 5:
                            P.mm(p4[:, 128:256], Nm[cur].v, NmT[cur].v, True, True)
                        yield
                        P.copy(Nm[nxt].v, p4[:, 0:128], eng="act")
                        if lev < 5:
                            P.copy(NmT[nxt].v, p4[:, 128:256], eng="dve")
                        yield
                        p5 = next_pb()
                        P.mm(p5[:, 0:128], Nm[nxt].v, PTm[cur].v, True, True)
                        yield
                        P.tt(PTm[nxt].v, p5[:, 0:128], PTm[cur].v, ALU.add)
                        yield
                        cur = nxt
                    ck(9)
                    PT = PTm[cur]
                    p6 = next_pb()
                    P.mm(p6[:, 0:128], PT.v, vb.v, True, True)
                    P.mm(p6[:, 128:256], kbg.v, PT.v, True, True)
                    yield
                    P.copy(u_sb.v, p6[:, 0:128], eng="act")
                    P.copy(wT.v, p6[:, 128:256], eng="dve")
                    yield
                    p7 = next_pb()
                    P.mm(p7[:, 0:128], wT.v, Sst.v, True, True)
                    yield
                    P.tt(vnew.v, u_sb.v, p7[:, 0:128], ALU.subtract)
                    yield
                    P.mm(p7[:, 128:256], qdT.v, Sst.v, True, False)
                    P.mm(p7[:, 128:256], QKmT.v, vnew.v, False, True)
                    P.mm(p7[:, 256:384], ktail.v, vnew.v, True, True)
                    yield
                    P.copy(o_sb.v, p7[:, 128:256], eng="act")
                    P.stt(Sst.v, Sst.v, bcol(sc_gtot), p7[:, 256:384], ALU.mult, ALU.add)
                    ck(10)
                    p8 = next_pb()
                    for kc in range(KC):
                        P.mm(p8[:, 0:128], hT[:, kc, csl], wq_h[3][:, kc, :], kc == 0, kc == KC - 1)
                    P.act(z_sb.v, p8[:, 0:128], AF.Silu)
                    yield
                    P.act(sqb.v, o_sb.v, AF.Square, accum=rn[:, 6:7])
                    rms_rstd(rn[:, 7:8], rn[:, 6:7], 128)
                    P.stt(og.v, o_sb.v, rn[:, 7:8], gainrep.v, ALU.mult, ALU.mult)
                    P.tt(og.v, og.v, z_sb.v, ALU.mult, eng="pool")
                    yield
                    P.tr(p8[:, 128:256], og.v, ident.v)
                    P.copy(ogT.v, p8[:, 128:256], eng="act")
                    yield
                    for hh in range(2):
                        ps = next_pb()
                        P.mm(ps.v, ogT.v, wout_f[:, hh * 512:(hh + 1) * 512], True, True)
                        P.tt(xs[c][:, hh * 512:(hh + 1) * 512], xs[c][:, hh * 512:(hh + 1) * 512], ps.v, ALU.add)

        for h0 in range(0, H, KH):
            gens = [head_gen(h0 + j, bufs[j]) for j in range(KH) if h0 + j < H]
            while gens:
                for g_ in list(gens):
                    try:
                        next(g_)
                    except StopIteration:
                        gens.remove(g_)

    TWO_PI = float(2 * np.pi)
    PI = float(np.pi)

    def layer1_mixer():
        B, A, G = modt
        P.scope()
        alloc_pieces()
        kTd = P.sc("kTd", [128, 2, S], BF16)
        vext = [P.sc("vext%d" % i, [128, 2, 65], BF16) for i in range(NT)]
        kgain = P.sc("kgain", [128, 64]); qgain = P.sc("qgain", [128, 64]); sinkrep = P.sc("sinkrep", [128, 16])
        posf = P.sc("posf", [128, NT]); posi = P.sc("posi", [128, NT], I32)
        cosb = P.sc("cosb", [128, NT, 8]); sinb = P.sc("sinb", [128, NT, 8])
        ang = P.sc("ang", [128, NT, 8]); angk = P.sc("angk", [128, NT, 8]); angi = P.sc("angi", [128, NT, 8], I32)
        invf = P.sc("invf", [128, NT, 8])
        kvw = P.sc("kvw", [128, KC, 256], BF16)
        kdup = P.sc("kdup", [128, 2, 128])
        kvt = P.sc("kvt", [128, 256]); qt = P.sc("qt", [128, D]); qsq = P.sc("qsq", [128, D])
        kvmod = [qt, qsq]
        hs = P.sc("hs", [128, 16]); rot = P.sc("rot", [128, 16, 16]); rtmp = P.sc("rtmp", [128, 16, 8])
        qTn = P.sc("qTn", [128, KC, 128], BF16)
        pexp = [P.sc("pexp%d" % i, [128, 8, 256], BF16) for i in range(2)]
        o_t = htmp; den = P.sc("den", [128, 16]); oT = P.sc("oT", [128, KC, 128], BF16)

        def rope_tables():
            P.dma("sp", posi.v, pos_in.v)
            P.copy(posf.v, posi.v)
            for f in range(8):
                P.memset(invf[:, :, f:f + 1], float(500000.0 ** (-(2 * f) / 16.0)), eng="pool")
            for shift, dst in ((0.0, sinb), (PI / 2, cosb)):
                P.tt(ang.v, invf.v, posf.v.rearrange("p (t o) -> p t o", o=1).bcast([128, NT, 8]), ALU.mult)
                if shift:
                    P.ts(ang.v, ang.v, shift, ALU.add)
                P.ts(angk.v, ang.v, 1.0 / TWO_PI, ALU.mult)
                P.copy(angi.v, angk.v)
                P.copy(angk.v, angi.v)
                P.stt(ang.v, angk.v, -TWO_PI, ang.v, ALU.mult, ALU.add)
                P.ts(angk.v, ang.v, PI, ALU.is_gt)
                P.stt(ang.v, angk.v, -TWO_PI, ang.v, ALU.mult, ALU.add)
                P.ts(angk.v, ang.v, -PI, ALU.is_lt)
                P.stt(ang.v, angk.v, TWO_PI, ang.v, ALU.mult, ALU.add)
                P.ts(ang.v, ang.v, PI, ALU.min, -PI, ALU.max)
                P.act(dst.v, ang.v, AF.Sin)

        def rms_rope(src, nh, gain, i, dst):
            s3 = src.rearrange("p (h d) -> p h d", d=64)
            d3 = dst.rearrange("p (h d) -> p h d", d=64)
            q3 = qsq[:, 0:nh * 64].rearrange("p (h d) -> p h d", d=64)
            P.tt(qsq[:, 0:nh * 64], src, src, ALU.mult, eng="pool")
            P.I("dve", lambda e: e.reduce_sum(out=hs[:, 0:nh].ap, in_=q3.ap, axis=mybir.AxisListType.X), [q3], [hs.v])
            rms_rstd(hs[:, 0:nh], hs[:, 0:nh], 64)
            P.tt(d3, s3, hs[:, 0:nh].rearrange("p (h o) -> p h o", o=1).bcast([128, nh, 64]), ALU.mult)
            P.tt(d3, d3, gain.v.rearrange("p (o d) -> p o d", o=1).bcast([128, nh, 64]), ALU.mult)
            cb = cosb[:, i, :].rearrange("p (o f) -> p o f", o=1).bcast([128, nh, 8])
            sb_ = sinb[:, i, :].rearrange("p (o f) -> p o f", o=1).bcast([128, nh, 8])
            x1, x2 = d3[:, :, 0:8], d3[:, :, 8:16]
            r = rot[:, 0:nh, :]
            P.tt(r[:, :, 0:8], x1, cb, ALU.mult)
            P.tt(rtmp[:, 0:nh, :], x2, sb_, ALU.mult)
            P.tt(r[:, :, 0:8], r[:, :, 0:8], rtmp[:, 0:nh, :], ALU.subtract)
            P.tt(r[:, :, 8:16], x2, cb, ALU.mult)
            P.tt(rtmp[:, 0:nh, :], x1, sb_, ALU.mult)
            P.tt(r[:, :, 8:16], r[:, :, 8:16], rtmp[:, 0:nh, :], ALU.add)
            P.copy(d3[:, :, 0:16], r, eng="pool")

        compute_mod(kv_ada_w, kv_ada_b, 0, [kvmod[0][:, 0:512], kvmod[0][:, 512:1024], kvmod[1][:, 0:512], kvmod[1][:, 512:1024]])
        gain_fold(kvmod[1], kv_norm_gain)
        P.dma("pool", kvw.v, wview(kv_w))
        P.dma("sp", kgain.v, pbc(k_norm_gain)); P.dma("sp", qgain.v, pbc(q_norm_gain)); P.dma("sp", sinkrep.v, pbc(b_sinks))
        P.act(sinkrep.v, sinkrep.v, AF.Exp)
        rope_tables()
        norm_all(kvmod[1], kvmod[0])
        for i in range(NT):
            isl = slice(i * 128, (i + 1) * 128)
            ps = next_pb()
            for kc in range(KC):
                P.mm(ps[:, 0:256], hT[:, kc, isl], kvw[:, kc, :], kc == 0, kc == KC - 1)
            P.copy(kvt.v, ps[:, 0:256], eng="act")
            P.memset(vext[i].v, 1.0, eng="pool")
            P.copy(vext[i][:, :, 0:64], kvt[:, 128:256].rearrange("p (g d) -> p g d", g=2))
            rms_rope(kvt[:, 0:128], 2, kgain, i, qt[:, 0:128])
            k3 = qt[:, 0:128].rearrange("p (g d) -> p g d", g=2)
            P.copy(kdup[:, :, 0:64], k3, eng="pool")
            P.copy(kdup[:, :, 64:128], k3, eng="pool")
            ps = next_pb()
            for g in range(2):
                P.tr(ps[:, g * 128:(g + 1) * 128], kdup[:, g, :], ident.v)
            P.copy(kTd[:, :, isl], ps[:, 0:256].rearrange("p (g t) -> p g t", g=2), eng="act")
        norm_all(A, B)
        wq2 = [piece[0], piece[1]]; wo2 = [piece[2], piece[3]]
        for hh in range(2):
            P.dma("pool", wq2[hh].v, wview(b_w_q)[:, :, hh * 512:(hh + 1) * 512])
            P.dma("pool", wo2[hh].v, wview(b_w_out)[:, :, hh * 512:(hh + 1) * 512])
            for kc in range(KC):
                P.tt(wo2[hh][:, kc, :], wo2[hh][:, kc, :], G[:, hh * 512:(hh + 1) * 512], ALU.mult, eng="pool")
        m3 = m01.v.rearrange("p (o k) -> p o k", o=1).bcast([128, 8, 256])
        for n in range(NT):
            nsl = slice(n * 128, (n + 1) * 128)
            psl = slice((n - 1) * 128, n * 128) if n > 0 else nsl
            for hh in range(2):
                ps = next_pb()
                for kc in range(KC):
                    P.mm(ps.v, hT[:, kc, nsl], wq2[hh][:, kc, :], kc == 0, kc == KC - 1)
                P.copy(qt[:, hh * 512:(hh + 1) * 512], ps.v, eng="act")
            rms_rope(qt.v, 16, qgain, n, hrow.v)
            transpose8(hrow.v, [(qTn.v, evac_eng())])
            for g in range(2):
                pe_ = pexp[g]
                for quad in range(2):
                    pss = [next_pb(), next_pb()]
                    for jj in range(4):
                        j = quad * 4 + jj
                        hq = g * 8 + j
                        kc, r0 = hq // 2, (hq % 2) * 64
                        ps = pss[jj % 2]
                        c0 = (jj // 2) * 256
                        P.mm(ps[:, c0:c0 + 128], kTd[r0:r0 + 64, g, psl], qTn[r0:r0 + 64, kc, :], True, True)
                        P.mm(ps[:, c0 + 128:c0 + 256], kTd[r0:r0 + 64, g, nsl], qTn[r0:r0 + 64, kc, :], True, True)
                    for jj in range(4):
                        c0 = (jj // 2) * 256
                        P.act(pe_[:, quad * 4 + jj, :], pss[jj % 2][:, c0:c0 + 256], AF.Exp, scale=0.125)
                P.tt(pe_.v, pe_.v, m3, ALU.mult)
                pos_ = [next_pb(), next_pb()]
                for j in range(8):
                    oc = pos_[j // 4][:, (j % 4) * 65:(j % 4 + 1) * 65]
                    if n > 0:
                        P.mm(oc, pe_[:, j, 0:128], vext[n - 1][:, g, :], True, False)
                        P.mm(oc, pe_[:, j, 128:256], vext[n][:, g, :], False, True)
                    else:
                        P.mm(oc, pe_[:, j, 128:256], vext[n][:, g, :], True, True)
                for q4 in range(2):
                    po3 = pos_[q4][:, 0:260].rearrange("p (j e) -> p j e", e=65)
                    dsl = slice(g * 8 + q4 * 4, g * 8 + q4 * 4 + 4)
                    P.tt(den[:, dsl].rearrange("p (j o) -> p j o", o=1), po3[:, :, 64:65],
                         sinkrep[:, dsl].rearrange("p (j o) -> p j o", o=1), ALU.add)
                    P.recip(den[:, dsl], den[:, dsl])
                    P.tt(o_t[:, (g * 8 + q4 * 4) * 64:(g * 8 + q4 * 4 + 4) * 64].rearrange("p (j d) -> p j d", d=64),
                         po3[:, :, 0:64], den[:, dsl].rearrange("p (j o) -> p j o", o=1).bcast([128, 4, 64]), ALU.mult)
            transpose8(o_t.v, [(oT.v, evac_eng())])
            for hh in range(2):
                ps = next_pb()
                for kc in range(KC):
                    P.mm(ps.v, oT[:, kc, :], wo2[hh][:, kc, :], kc == 0, kc == KC - 1)
                P.tt(xs[n][:, hh * 512:(hh + 1) * 512], xs[n][:, hh * 512:(hh + 1) * 512], ps.v, ALU.add)

    stages = [("mod0a", lambda: layer_mod(0, 0)), ("gdn", gdn), ("mod0b", lambda: layer_mod(0, 1)), ("moe0", lambda: moe(0)),
              ("mod1a", lambda: layer_mod(1, 0)), ("attn", layer1_mixer), ("mod1b", lambda: layer_mod(1, 1)),
              ("moe1", lambda: moe(1))]
    try:
        for name, fn in stages:
            fn()
            if stop_after == name:
                break
    except _Cut:
        pass
    P.scope()
    for i in range(NT):
        P.dma("sp", y_out[i * 128:(i + 1) * 128, :], xs[i].v, key=xs[i])
    P.emit()
    return nc, P


def make_in_maps(inputs, S=2048, NE=32):
    f = lambda a: np.ascontiguousarray(np.asarray(a, dtype=np.float32))
    B = inputs["x"].shape[0]
    NT = S // 128
    up_b = np.asarray(inputs["up_b"], dtype=np.float32)
    shared = {
        "ada_w": f(inputs["ada_w"]), "ada_b": f(inputs["ada_b"]), "norm_gain": f(inputs["norm_gain"]).reshape(4, D),
        "a_w_in": f(inputs["a_w_in"][0]), "a_convT": f(np.asarray(inputs["a_conv"][0]).T), "a_log": f(inputs["a_log"][0]),
        "a_dt_bias": f(inputs["a_dt_bias"][0]), "a_out_gain": f(inputs["a_out_gain"][0]), "a_w_out": f(inputs["a_w_out"][0]),
        "kv_ada_w": f(inputs["kv_ada_w"]), "kv_ada_b": f(inputs["kv_ada_b"]).reshape(1, -1),
        "kv_norm_gain": f(inputs["kv_norm_gain"]).reshape(1, -1),
        "kv_w": f(inputs["kv_w"]), "k_norm_gain": f(inputs["k_norm_gain"]), "b_w_q": f(inputs["b_w_q"][0]),
        "q_norm_gain": f(inputs["q_norm_gain"][0]), "b_sinks": f(inputs["b_sinks"][0]), "b_w_out": f(inputs["b_w_out"][0]),
        "router_w": f(inputs["router_w"]), "router_b": f(inputs["router_b"]), "up_w": f(inputs["up_w"]),
        "up_bT": f(up_b.reshape(2, up_b.shape[1], 16, 128).transpose(0, 1, 3, 2)),
        "down_w": f(inputs["down_w"]), "down_b": f(inputs["down_b"]),
    }
    maps = []
    for b in range(B):
        m = dict(shared)
        m["x"] = f(inputs["x"][b])
        m["cT"] = f(np.asarray(inputs["c"][b]).reshape(KC, 128).T)
        m["pos"] = np.ascontiguousarray(np.asarray(inputs["positions"][b], dtype=np.int32).reshape(NT, 128).T)
        maps.append(m)
    return maps


_CACHE = {}


def kernel(**inputs):
    if "nc" not in _CACHE:
        _CACHE["nc"] = build()[0]
    nc = _CACHE["nc"]
    maps = make_in_maps(inputs)
    res = run_bass_kernel_spmd(nc, maps, core_ids=list(range(len(maps))))
    return np.stack([np.asarray(r["y"], dtype=np.float32) for r in res.results], axis=0)
```
